# Optimizing a Trainium2 kernel written in Bass

```python
import math
import jax, jax.numpy as jnp
from jax import lax
import numpy as np

D_MODEL = 1024
BATCH = 8
SEQ = 2048
DEPTH = 1

GDN_HEADS = 8
GDN_DK = 128
GDN_DV = 128
GDN_CONV = 4
GDN_CHUNK = 64
DIL_PATTERN = ((128, 1), (512, 4), (2048, 16))
DIL_HEADS_PER_GROUP = 4
DIL_HD = 128
DIL_BLOCK = 128
N_EXPERTS = 64
TOP_K = 8
N_GROUPS = 8
TOPK_GROUPS = 4
D_EXPERT = 256
D_SHARED = 256
ROUTE_SCALE = 2.5
MOE_TOKEN_BLOCK = 512
EPS = 1e-6

GDN_QK_W = GDN_HEADS * GDN_DK
GDN_V_W = GDN_HEADS * GDN_DV
N_DIL_GROUPS = len(DIL_PATTERN)
DIL_HEADS = N_DIL_GROUPS * DIL_HEADS_PER_GROUP
DIL_W = DIL_HEADS * DIL_HD
DIL_OUT_W = DIL_HEADS_PER_GROUP * DIL_HD
SPLIT_WIDTHS = (GDN_QK_W, GDN_QK_W, GDN_V_W, GDN_V_W, GDN_HEADS, GDN_HEADS,
                DIL_W, DIL_W, DIL_W, D_MODEL, D_MODEL)
IN_W = sum(SPLIT_WIDTHS)
SPLIT_OFFSETS = tuple(sum(SPLIT_WIDTHS[:i + 1]) for i in range(len(SPLIT_WIDTHS) - 1))

kernel_name = "hybrid_gdn_dilated_moe_adaln"


def _rms(t, g):
    t32 = t.astype(jnp.float32)
    return t32 * lax.rsqrt(jnp.mean(t32 * t32, axis=-1, keepdims=True) + EPS) * g.astype(jnp.float32)


def _l2norm(t):
    return t * lax.rsqrt(jnp.sum(t * t, axis=-1, keepdims=True) + EPS)


def causal_depthwise_conv(x, w):
    K, C = w.shape
    return lax.conv_general_dilated(x, w[:, None, :], window_strides=(1,), padding=((K - 1, 0),),
                                    dimension_numbers=('NWC', 'WIO', 'NWC'), feature_group_count=C)


def chunk_gated_delta_rule(q, k, v, g, beta):
    B, H, S, dk = q.shape
    dv = v.shape[-1]
    C = GDN_CHUNK
    N = S // C
    q = q.reshape(B, H, N, C, dk)
    k = k.reshape(B, H, N, C, dk)
    v = v.reshape(B, H, N, C, dv)
    g = g.reshape(B, H, N, C)
    beta = beta.reshape(B, H, N, C)
    gc = jnp.cumsum(g, axis=-1)
    incl = jnp.tril(jnp.ones((C, C), bool))
    strict = jnp.tril(jnp.ones((C, C), bool), -1)
    decay = jnp.exp(jnp.where(incl, gc[..., :, None] - gc[..., None, :], -jnp.inf))
    k_beta = k * beta[..., None]
    a_mat = jnp.where(strict, jnp.einsum('bhnid,bhnjd->bhnij', k_beta, k) * decay, 0.0)
    lmat = a_mat + jnp.eye(C, dtype=a_mat.dtype)
    rhs = jnp.concatenate([v * beta[..., None], k_beta * jnp.exp(gc)[..., None]], axis=-1)
    sol = lax.linalg.triangular_solve(lmat, rhs, left_side=True, lower=True, unit_diagonal=True)
    u, w = sol[..., :dv], sol[..., dv:]
    qk = jnp.einsum('bhnid,bhnjd->bhnij', q, k) * decay
    q_dec = q * jnp.exp(gc)[..., None]
    k_dec = k * jnp.exp(gc[..., -1:] - gc)[..., None]
    g_tot = jnp.exp(gc[..., -1])

    def step(state, xs):
        u_n, w_n, qk_n, qd_n, kd_n, gt_n = xs
        v_new = u_n - jnp.einsum('bhck,bhkv->bhcv', w_n, state)
        o_n = jnp.einsum('bhck,bhkv->bhcv', qd_n, state) + jnp.einsum('bhij,bhjv->bhiv', qk_n, v_new)
        state = state * gt_n[..., None, None] + jnp.einsum('bhck,bhcv->bhkv', kd_n, v_new)
        return state, o_n

    xs = tuple(jnp.moveaxis(t, 2, 0) for t in (u, w, qk, q_dec, k_dec, g_tot))
    state0 = jnp.zeros((B, H, dk, dv), q.dtype)
    _, o = lax.scan(step, state0, xs)
    return jnp.moveaxis(o, 0, 2).reshape(B, H, S, dv)


def gated_deltanet(q, k, v, z, b, a, conv_w, a_log, dt_bias, norm_g):
    B, S, _ = q.shape
    f32 = jnp.float32
    qkv = jnp.concatenate([q, k, v], axis=-1).astype(f32)
    qkv = jax.nn.silu(causal_depthwise_conv(qkv, conv_w.astype(f32)))
    q, k, v = jnp.split(qkv, [GDN_QK_W, 2 * GDN_QK_W], axis=-1)

    def heads(t, d):
        return t.reshape(B, S, GDN_HEADS, d).transpose(0, 2, 1, 3)

    q = _l2norm(heads(q, GDN_DK)) * (GDN_DK ** -0.5)
    k = _l2norm(heads(k, GDN_DK))
    v = heads(v, GDN_DV)
    beta = jax.nn.sigmoid(b.astype(f32)).transpose(0, 2, 1)
    g = (-jnp.exp(a_log.astype(f32)) * jax.nn.softplus(a.astype(f32) + dt_bias.astype(f32))).transpose(0, 2, 1)
    o = chunk_gated_delta_rule(q, k, v, g, beta).transpose(0, 2, 1, 3)
    zg = jax.nn.silu(z.astype(f32).reshape(B, S, GDN_HEADS, GDN_DV))
    o = _rms(o, norm_g) * zg
    return o.reshape(B, S, GDN_V_W)


def dilated_window_attention(q, k, v, window, dilation):
    B, S, h, e = q.shape
    L = S // dilation
    W = window // dilation
    blk = math.gcd(L, DIL_BLOCK)
    nb = L // blk

    def sub(t):
        return t.reshape(B, L, dilation, h, e).transpose(0, 2, 3, 1, 4)

    qs = sub(q).reshape(B, dilation, h, nb, blk, e)
    key_idx = np.arange(nb)[:, None] * blk + np.arange(blk + W)[None, :]
    pad = ((0, 0), (0, 0), (0, 0), (W, 0), (0, 0))
    ks = jnp.pad(sub(k), pad)[:, :, :, key_idx]
    vs = jnp.pad(sub(v), pad)[:, :, :, key_idx]
    dist = np.arange(blk)[:, None] + W - np.arange(blk + W)[None, :]
    valid = (dist >= 0) & (dist <= W) & ((key_idx[:, None, :] - W) >= 0)
    s = jnp.einsum('bdhnqe,bdhnke->bdhnqk', qs, ks) * (e ** -0.5)
    s = jnp.where(valid, s, -jnp.inf)
    m = jnp.max(s, axis=-1, keepdims=True)
    p = jnp.exp(s - m)
    den = jnp.sum(p, axis=-1, keepdims=True)
    o = jnp.einsum('bdhnqk,bdhnke->bdhnqe', p, vs) / den
    lse = (m + jnp.log(den))[..., 0]
    o = o.reshape(B, dilation, h, L, e).transpose(0, 3, 1, 2, 4).reshape(B, S, h, e)
    lse = lse.reshape(B, dilation, h, L).transpose(0, 3, 1, 2).reshape(B, S, h)
    return o, lse


def dilated_mixture(q, k, v, q_norm_g, k_norm_g):
    B, S, _ = q.shape
    q = _rms(q.reshape(B, S, DIL_HEADS, DIL_HD), q_norm_g)
    k = _rms(k.reshape(B, S, DIL_HEADS, DIL_HD), k_norm_g)
    v = v.reshape(B, S, DIL_HEADS, DIL_HD).astype(jnp.float32)
    outs, lses = [], []
    for gi, (win, dil) in enumerate(DIL_PATTERN):
        hs = slice(gi * DIL_HEADS_PER_GROUP, (gi + 1) * DIL_HEADS_PER_GROUP)
        o, lse = dilated_window_attention(q[:, :, hs], k[:, :, hs], v[:, :, hs], win, dil)
        outs.append(o)
        lses.append(lse)
    wts = jax.nn.softmax(jnp.stack(lses), axis=0)
    y = jnp.einsum('gbsh,gbshe->bshe', wts, jnp.stack(outs))
    return y.reshape(B, S, DIL_OUT_W)


def moe_ffn(h, w_router, router_bias, w_gate, w_up, w_down, ws_gate, ws_up, ws_down):
    B, S, D = h.shape
    T = B * S
    f32 = jnp.float32
    ht = h.reshape(T, D)
    scores = jax.nn.sigmoid(ht.astype(f32) @ w_router.astype(f32))
    sel = scores + router_bias.astype(f32)
    grp = sel.reshape(T, N_GROUPS, N_EXPERTS // N_GROUPS)
    grp_score = jnp.sum(lax.top_k(grp, 2)[0], axis=-1)
    _, gidx = lax.top_k(grp_score, TOPK_GROUPS)
    gmask = jnp.sum(jax.nn.one_hot(gidx, N_GROUPS, dtype=f32), axis=1)
    emask = jnp.repeat(gmask, N_EXPERTS // N_GROUPS, axis=1) > 0
    _, eidx = lax.top_k(jnp.where(emask, sel, -jnp.inf), TOP_K)
    wk = jnp.take_along_axis(scores, eidx, axis=-1)
    wk = wk / jnp.sum(wk, axis=-1, keepdims=True) * ROUTE_SCALE
    combine = jnp.einsum('tk,tke->te', wk, jax.nn.one_hot(eidx, N_EXPERTS, dtype=f32)).astype(h.dtype)
    tb = math.gcd(T, MOE_TOKEN_BLOCK)

    def block(args):
        hb, cb = args
        a = jnp.einsum('td,edf->tef', hb, w_gate)
        u = jnp.einsum('td,edf->tef', hb, w_up)
        return jnp.einsum('tef,efd->td', jax.nn.silu(a) * u * cb[..., None], w_down)

    routed = lax.map(block, (ht.reshape(T // tb, tb, D), combine.reshape(T // tb, tb, N_EXPERTS))).reshape(T, D)
    shared = (jax.nn.silu(ht @ ws_gate) * (ht @ ws_up)) @ ws_down
    return (routed + shared).reshape(B, S, D)


def setup_inputs(seed: int = 0) -> dict:
    key = jax.random.key(seed)
    ks = jax.random.split(key, 24)
    L, D, E = DEPTH, D_MODEL, N_EXPERTS
    nrm = lambda k, shape, scale: jax.random.normal(k, shape, jnp.float32) * scale
    dt = jnp.exp(jax.random.uniform(ks[7], (L, GDN_HEADS), jnp.float32, math.log(1e-3), math.log(1e-1)))
    return {
        "x": nrm(ks[0], (BATCH, SEQ, D), 1.0),
        "c": nrm(ks[1], (BATCH, D), 1.0),
        "w_ada": nrm(ks[2], (L, D, 6 * D), 0.5 * D ** -0.5),
        "b_ada": nrm(ks[3], (L, 6 * D), 0.02),
        "g_mix": 1.0 + nrm(ks[4], (L, D), 0.05),
        "w_in": nrm(ks[5], (L, D, IN_W), D ** -0.5),
        "gdn_conv_w": nrm(ks[6], (L, GDN_CONV, 2 * GDN_QK_W + GDN_V_W), 0.5),
        "gdn_a_log": jnp.log(jax.random.uniform(ks[8], (L, GDN_HEADS), jnp.float32, 1.0, 16.0)),
        "gdn_dt_bias": dt + jnp.log(-jnp.expm1(-dt)),
        "gdn_norm_g": 1.0 + nrm(ks[9], (L, GDN_DV), 0.05),
        "dil_q_norm_g": 1.0 + nrm(ks[10], (L, DIL_HD), 0.05),
        "dil_k_norm_g": 1.0 + nrm(ks[11], (L, DIL_HD), 0.05),
        "w_up_gdn": nrm(ks[12], (L, GDN_V_W, D), GDN_V_W ** -0.5),
        "w_up_dil": nrm(ks[13], (L, DIL_OUT_W, D), DIL_OUT_W ** -0.5),
        "w_out": nrm(ks[14], (L, D, D), D ** -0.5),
        "g_ffn": 1.0 + nrm(ks[15], (L, D), 0.05),
        "w_router": nrm(ks[16], (L, D, E), D ** -0.5),
        "router_bias": nrm(ks[17], (L, E), 0.01),
        "w_exp_gate": nrm(ks[18], (L, E, D, D_EXPERT), D ** -0.5),
        "w_exp_up": nrm(ks[19], (L, E, D, D_EXPERT), D ** -0.5),
        "w_exp_down": nrm(ks[20], (L, E, D_EXPERT, D), D_EXPERT ** -0.5),
        "w_sh_gate": nrm(ks[21], (L, D, D_SHARED), D ** -0.5),
        "w_sh_up": nrm(ks[22], (L, D, D_SHARED), D ** -0.5),
        "w_sh_down": nrm(ks[23], (L, D_SHARED, D), D_SHARED ** -0.5),
    }


def reference(x, c, w_ada, b_ada, g_mix, w_in, gdn_conv_w, gdn_a_log, gdn_dt_bias, gdn_norm_g,
              dil_q_norm_g, dil_k_norm_g, w_up_gdn, w_up_dil, w_out, g_ffn, w_router, router_bias,
              w_exp_gate, w_exp_up, w_exp_down, w_sh_gate, w_sh_up, w_sh_down):
    cs = jax.nn.silu(c)
    for l in range(DEPTH):
        mod = (cs @ w_ada[l] + b_ada[l])[:, None, :]
        sh1, sc1, gt1, sh2, sc2, gt2 = jnp.split(mod, 6, axis=-1)
        h = _rms(x, g_mix[l]).astype(x.dtype) * (1.0 + sc1) + sh1
        proj = h @ w_in[l]
        (gq, gk, gv, gz, gb, ga, dil_q, dil_k, dil_v, gate_a, gate_b) = jnp.split(proj, SPLIT_OFFSETS, axis=-1)
        y_a = gated_deltanet(gq, gk, gv, gz, gb, ga, gdn_conv_w[l], gdn_a_log[l], gdn_dt_bias[l],
                             gdn_norm_g[l]).astype(x.dtype)
        y_b = dilated_mixture(dil_q, dil_k, dil_v, dil_q_norm_g[l], dil_k_norm_g[l]).astype(x.dtype)
        merged = jax.nn.sigmoid(gate_a) * (y_a @ w_up_gdn[l]) + jax.nn.sigmoid(gate_b) * (y_b @ w_up_dil[l])
        x = x + gt1 * (merged @ w_out[l])
        h2 = _rms(x, g_ffn[l]).astype(x.dtype) * (1.0 + sc2) + sh2
        x = x + gt2 * moe_ffn(h2, w_router[l], router_bias[l], w_exp_gate[l], w_exp_up[l], w_exp_down[l],
                              w_sh_gate[l], w_sh_up[l], w_sh_down[l])
    return x
```

```python
import numpy as np
import concourse.bass as bass
import concourse.mybir as mybir
from concourse.bass_utils import run_bass_kernel_spmd

F32 = mybir.dt.float32
BF = mybir.dt.bfloat16
AF = mybir.ActivationFunctionType
ALU = mybir.AluOpType
AX = mybir.AxisListType

ENGS = ("pe", "act", "dve", "pool", "sp")

T = 2048
D = 1024
NT = 16
KC = 8
IN_W = 10768
EPS = 1e-6
N_EXP = 64
OFF_GQ, OFF_GK, OFF_GV, OFF_GZ, OFF_GB, OFF_GA = 0, 1024, 2048, 3072, 4096, 4104
OFF_DQ, OFF_DK, OFF_DV, OFF_GA_GATE, OFF_GB_GATE = 4112, 5648, 7184, 8720, 9744


class Sched:
    def __init__(self, nc):
        self.nc = nc
        self.eng = {"pe": nc.tensor, "act": nc.scalar, "dve": nc.vector,
                    "pool": nc.gpsimd, "sp": nc.sync}
        self.ops = {e: [] for e in ENGS}
        self.order = []
        self.fsz = {}
        self.kind = {}
        self.recs = {}
        self.dma_keys = {}
        self.dma_last = {}
        self.pending = {}

    def barrier(self):
        last = set()
        for e in ENGS:
            if self.ops[e]:
                last.add((e, len(self.ops[e]) - 1))
        for k, v in self.dma_last.items():
            last.add(v)
        for (de, di) in last:
            self.ops[de][di]["signal"] = True
        for e in ENGS:
            self.pending[e] = set(last) | self.pending.get(e, set())

    def sb(self, name, shape, dtype, off=None):
        if off is None:
            raise RuntimeError("explicit offset required")
        t = self.nc.alloc_sbuf_tensor_at(name, list(shape), dtype, offset=off)
        f = 1
        for s in shape[1:]:
            f *= s
        self.fsz[t.name] = f
        self.kind[t.name] = "sb"
        self.recs[t.name] = []
        return t

    def ps(self, name, shape, dtype):
        t = self.nc.alloc_psum_tensor(name, list(shape), dtype)
        f = 1
        for s in shape[1:]:
            f *= s
        self.fsz[t.name] = f
        self.kind[t.name] = "ps"
        self.recs[t.name] = []
        return t

    def box(self, ap):
        name = ap.tensor.name
        if name not in self.fsz:
            return None
        f = self.fsz[name]
        if self.kind[name] == "ps":
            return (name, 0, 128, 0, f)
        off = ap.offset
        dims = ap.ap
        plo = off // f
        phi = plo + dims[0][1]
        flo = off % f
        ext = 1
        for st, cnt in dims[1:]:
            ext += (cnt - 1) * abs(st)
        return (name, plo, phi, flo, flo + ext)

    def add(self, e, fn, reads=(), writes=(), dma_key=None):
        idx = len(self.ops[e])
        op = dict(fn=fn, deps=set(), dma_key=dma_key, signal=False, val=None)
        rb = [self.box(a) for a in reads]
        wb = [self.box(a) for a in writes]
        deps = set()
        for b in rb:
            if b is None:
                continue
            name, plo, phi, flo, fhi = b
            isps = self.kind[name] == "ps"
            for r in self.recs[name]:
                if (r[6] or isps) and r[0] < phi and plo < r[1] and r[2] < fhi and flo < r[3]:
                    deps.add((r[4], r[5]))
        for b in wb:
            if b is None:
                continue
            name, plo, phi, flo, fhi = b
            for r in self.recs[name]:
                if r[0] < phi and plo < r[1] and r[2] < fhi and flo < r[3]:
                    deps.add((r[4], r[5]))
        for b in wb:
            if b is None:
                continue
            name, plo, phi, flo, fhi = b
            lst = self.recs[name]
            lst[:] = [r for r in lst if not (plo <= r[0] and r[1] <= phi and flo <= r[2] and r[3] <= fhi)]
            lst.append([plo, phi, flo, fhi, e, idx, True])
        for b in rb:
            if b is None:
                continue
            name, plo, phi, flo, fhi = b
            lst = self.recs[name]
            if self.kind[name] == "ps":
                lst[:] = [[plo, phi, flo, fhi, e, idx, True]]
            else:
                lst[:] = [r for r in lst if not ((not r[6]) and r[4] == e and dma_key is None and r[5] < idx
                                                 and self.ops[e][r[5]]["dma_key"] is None
                                                 and plo <= r[0] and r[1] <= phi and flo <= r[2] and r[3] <= fhi)]
                lst.append([plo, phi, flo, fhi, e, idx, False])
        if e in self.pending:
            deps |= self.pending.pop(e)
        if dma_key is not None and dma_key in self.dma_last:
            deps.add(self.dma_last[dma_key])
        fdeps = set()
        best = {}
        for (de, di) in deps:
            dop = self.ops[de][di]
            if de == e and e == "pe" and dop["dma_key"] is None and dma_key is None:
                continue
            if dop["dma_key"] is not None:
                fdeps.add((de, di))
            elif best.get(de, -1) < di:
                best[de] = di
        for de, di in best.items():
            fdeps.add((de, di))
        for (de, di) in fdeps:
            self.ops[de][di]["signal"] = True
        op["deps"] = fdeps
        if dma_key is not None:
            op["signal"] = True
            if dma_key not in self.dma_keys:
                self.dma_keys[dma_key] = dict(sem=None, count=0)
            self.dma_last[dma_key] = (e, idx)
        self.ops[e].append(op)
        self.order.append((e, idx))
        return (e, idx)

    def emit(self):
        nc = self.nc
        esem = {e: nc.alloc_semaphore("s_" + e) for e in ENGS}
        for i, (k, d) in enumerate(self.dma_keys.items()):
            d["sem"] = nc.alloc_semaphore("d_%d" % i)
        for e in ENGS:
            c = 0
            for op in self.ops[e]:
                if op["dma_key"] is not None:
                    d = self.dma_keys[op["dma_key"]]
                    d["count"] += 1
                    op["val"] = (d["sem"], 16 * d["count"])
                elif op["signal"]:
                    c += 1
                    op["val"] = (esem[e], c)
        waited = {e: {} for e in ENGS}
        for (e, idx) in self.order:
            op = self.ops[e][idx]
            eng = self.eng[e]
            need = {}
            for (de, di) in op["deps"]:
                sem, val = self.ops[de][di]["val"]
                key = sem.num
                if key not in need or need[key][1] < val:
                    need[key] = (sem, val)
            for key, (sem, val) in need.items():
                if waited[e].get(key, 0) >= val:
                    continue
                waited[e][key] = val
                eng.wait_ge(sem, val)
            ins = op["fn"](eng)
            if op["val"] is not None:
                sem, val = op["val"]
                ins.then_inc(sem, 16 if op["dma_key"] is not None else 1)

    def final_wait(self, e, ops):
        eng = self.eng[e]
        for (de, di) in ops:
            sem, val = self.ops[de][di]["val"]
            eng.wait_ge(sem, val)


def run_threads(gens):
    gens = list(gens)
    while gens:
        nxt = []
        for g in gens:
            try:
                next(g)
                nxt.append(g)
            except StopIteration:
                pass
        gens = nxt


def build(stage=99, dbg=()):
    nc = bass.Bass("TRN2", target_bir_lowering=False)
    S = Sched(nc)
    din = {}

    def DI(name, shape):
        din[name] = nc.dram_tensor(name, list(shape), F32, kind="ExternalInput")
        return din[name]

    x_d = DI("x", [T, D])
    c_d = DI("c", [D])
    w_ada_d = DI("w_ada", [D, 6 * D])
    b_ada_d = DI("b_ada", [6 * D])
    g_mix_d = DI("g_mix", [D])
    w_in_d = DI("w_in", [D, IN_W])
    conv_w_d = DI("gdn_conv_w", [4, 3072])
    a_log_d = DI("gdn_a_log", [8])
    dt_bias_d = DI("gdn_dt_bias", [8])
    gdn_ng_d = DI("gdn_norm_g", [128])
    dil_qg_d = DI("dil_q_norm_g", [128])
    dil_kg_d = DI("dil_k_norm_g", [128])
    w_upg_d = DI("w_up_gdn", [D, D])
    w_upd_d = DI("w_up_dil", [512, D])
    w_out_d = DI("w_out", [D, D])
    g_ffn_d = DI("g_ffn", [D])
    w_r_d = DI("w_router", [D, 64])
    r_bias_d = DI("router_bias", [64])
    w_eg_d = DI("w_exp_gate", [64, D, 256])
    w_eu_d = DI("w_exp_up", [64, D, 256])
    w_ed_d = DI("w_exp_down", [64, 256, D])
    w_sg_d = DI("w_sh_gate", [D, 256])
    w_su_d = DI("w_sh_up", [D, 256])
    w_sd_d = DI("w_sh_down", [256, D])
    out_d = nc.dram_tensor("out", [T, D], F32, kind="ExternalOutput")
    dbg_out = {}
    finals = []

    def mm(out, lhsT, rhs, start=True, stop=True):
        S.add("pe", lambda e: e.matmul(out, lhsT=lhsT, rhs=rhs, start=start, stop=stop),
              reads=[lhsT, rhs], writes=[out])

    def tr(out, in_, idn):
        S.add("pe", lambda e: e.transpose(out=out, in_=in_, identity=idn),
              reads=[in_, idn], writes=[out])

    def act(out, in_, func, bias=None, scale=None, accum=None, eng="act"):
        rd = [in_]
        kw = {}
        if bias is not None:
            kw["bias"] = bias
            if not isinstance(bias, float):
                rd.append(bias)
        if scale is not None:
            kw["scale"] = scale
            if not isinstance(scale, float):
                rd.append(scale)
        wr = [out]
        if accum is not None:
            kw["accum_out"] = accum
            wr.append(accum)
        S.add(eng, lambda e: e.activation(out=out, in_=in_, func=func, **kw), reads=rd, writes=wr)

    def ts(out, in0, s1, s2=None, op0=ALU.mult, op1=None, eng="dve"):
        rd = [in0]
        if not isinstance(s1, float):
            rd.append(s1)
        if s2 is not None and not isinstance(s2, float):
            rd.append(s2)
        if op1 is None:
            S.add(eng, lambda e: e.tensor_scalar(out=out, in0=in0, scalar1=s1, scalar2=None, op0=op0),
                  reads=rd, writes=[out])
        else:
            S.add(eng, lambda e: e.tensor_scalar(out=out, in0=in0, scalar1=s1, scalar2=s2, op0=op0, op1=op1),
                  reads=rd, writes=[out])

    def tt(out, in0, in1, op, eng="dve"):
        S.add(eng, lambda e: e.tensor_tensor(out=out, in0=in0, in1=in1, op=op), reads=[in0, in1], writes=[out])

    def stt(out, in0, scalar, in1, op0, op1, eng="dve"):
        rd = [in0, in1]
        if not isinstance(scalar, float):
            rd.append(scalar)
        S.add(eng, lambda e: e.scalar_tensor_tensor(out=out, in0=in0, scalar=scalar, in1=in1, op0=op0, op1=op1),
              reads=rd, writes=[out])

    def cp(out, in_, eng="dve"):
        if eng == "act":
            act(out, in_, AF.Copy)
        else:
            S.add(eng, lambda e: e.tensor_copy(out=out, in_=in_), reads=[in_], writes=[out])

    def memset(ap, v, eng="pool"):
        S.add(eng, lambda e: e.memset(ap, v), writes=[ap])

    def recip(out, in_):
        S.add("dve", lambda e: e.reciprocal(out=out, in_=in_), reads=[in_], writes=[out])

    def dma(q, out, in_, key):
        return S.add(q, lambda e: e.dma_start(out=out, in_=in_), reads=[in_], writes=[out], dma_key=key)

    def dump(name, ap, shape):
        t = nc.dram_tensor("dbg_" + name, list(shape), ap.dtype, kind="ExternalOutput")
        dbg_out[name] = t
        finals.append(dma("sp", t.ap(), ap, "dbg_" + name))

    def finish():
        S.emit()
        S.final_wait("sp", finals)
        return nc, dbg_out

    class Arena:
        def __init__(self, base, limit):
            self.base, self.limit, self.cur = base, limit, base

        def a(self, name, shape, dtype):
            n = 1
            for s_ in shape[1:]:
                n *= s_
            nb = n * (4 if dtype == F32 else 2)
            nb = (nb + 31) // 32 * 32
            off = self.cur
            self.cur += nb
            assert self.cur <= self.limit, ("arena overflow", name, self.cur, self.limit)
            return S.sb(name, shape, dtype, off=off)

    KB = 1024
    B0 = 17 * KB
    CA = Arena(B0, B0 + 16 * KB)
    WA = Arena(B0 + 96 * KB, B0 + 207 * KB)

    banks = [S.ps("bank%d" % i, [128, 512], F32) for i in range(8)]
    bank_ctr = [0]

    held = set()

    def bank():
        for _ in range(8):
            i = bank_ctr[0] % 8
            bank_ctr[0] += 1
            if i not in held:
                return banks[i]
        raise RuntimeError("no free PSUM bank")

    def bacq():
        for _ in range(8):
            i = bank_ctr[0] % 8
            bank_ctr[0] += 1
            if i not in held:
                held.add(i)
                assert len(held) <= 8, "too many PSUM banks held"
                return banks[i]
        raise RuntimeError("no free PSUM bank")

    def brel(b):
        held.discard(banks.index(b))

    def bf(ap):
        return ap.bitcast(BF)

    ident = CA.a("ident", [128, 128], F32)
    identb = CA.a("identb", [128, 128], BF)
    ones_f = CA.a("ones_f", [128, 128], F32)
    ones_b = CA.a("ones_b", [128, 128], BF)
    memset(ident[:], 1.0)
    S.add("pool", lambda e: e.affine_select(out=ident[:], in_=ident[:], pattern=[[-1, 128]], compare_op=ALU.is_equal,
                                            fill=0.0, base=0, channel_multiplier=1), reads=[ident[:]], writes=[ident[:]])
    cp(identb[:], ident[:])
    memset(ones_f[:], 1.0)
    memset(ones_b[:], 1.0)
    blk2 = CA.a("blk2", [128, 128], F32)
    tri2 = CA.a("tri2", [128, 128], F32)
    mask2 = CA.a("mask2", [128, 2, 128], F32)
    half01 = CA.a("half01", [128, 2, 128], F32)
    memset(blk2[:], 0.0)
    memset(blk2[0:64, 0:64], 1.0)
    memset(blk2[64:128, 64:128], 1.0)
    S.add("pool", lambda e: e.affine_select(out=tri2[:], in_=blk2[:], pattern=[[1, 128]], compare_op=ALU.is_ge,
                                            fill=0.0, base=0, channel_multiplier=-1), reads=[blk2[:]], writes=[tri2[:]])
    cp(mask2[:, 0, :], tri2[:], eng="pool")
    S.add("pool", lambda e: e.affine_select(out=mask2[:, 1, :], in_=blk2[:], pattern=[[1, 128]], compare_op=ALU.is_ge,
                                            fill=0.0, base=-1, channel_multiplier=-1), reads=[blk2[:]], writes=[mask2[:, 1, :]])
    memset(half01[:], 0.0)
    memset(half01[0:64, 0, :], 1.0)
    memset(half01[64:128, 1, :], 1.0)
    amask = CA.a("amask", [128, 2, 128], BF)
    S.add("pool", lambda e: e.affine_select(out=amask[:, 0, :], in_=ones_b[:], pattern=[[-1, 128]], compare_op=ALU.is_ge,
                                            fill=0.0, base=0, channel_multiplier=1), reads=[ones_b[:]], writes=[amask[:, 0, :]])
    S.add("pool", lambda e: e.affine_select(out=amask[:, 1, :], in_=ones_b[:], pattern=[[1, 128]], compare_op=ALU.is_ge,
                                            fill=0.0, base=0, channel_multiplier=-1), reads=[ones_b[:]], writes=[amask[:, 1, :]])

    stg = WA.a("stg", [72, 128], F32)
    dma("sp", stg[0:8, :], c_d.ap().rearrange("(k p) -> k p", p=128), "st0")
    dma("sp", stg[8:16, :], g_mix_d.ap().rearrange("(k p) -> k p", p=128), "st1")
    dma("sp", stg[16:24, :], g_ffn_d.ap().rearrange("(k p) -> k p", p=128), "st2")
    dma("sp", stg[24:72, :], b_ada_d.ap().rearrange("(k p) -> k p", p=128), "st3")
    cols = CA.a("cols", [128, 72], F32)
    pb_ = bank()
    tr(pb_[:, 0:72], stg[:, :], ident[0:72, 0:72])
    cp(cols[:], pb_[:, 0:72])
    cs_b = CA.a("cs_b", [128, 8], BF)
    act(cs_b[:], cols[:, 0:8], AF.Silu)
    stg2 = WA.a("stg2", [96, 128], F32)
    dma("sp", stg2[:, :], conv_w_d.ap().rearrange("j (c p) -> (j c) p", p=128), "st4")
    convcol = CA.a("convcol", [128, 96], F32)
    pb_ = bank()
    tr(pb_[:, 0:96], stg2[:, :], ident[0:96, 0:96])
    cp(convcol[:], pb_[:, 0:96])
    rowc = CA.a("rowc", [128, 16 + 128 + 64], F32)
    dma("sp", rowc[:, 0:8], a_log_d.ap().partition_broadcast(128), "st5")
    dma("sp", rowc[:, 8:16], dt_bias_d.ap().partition_broadcast(128), "st6")
    dma("sp", rowc[:, 16:144], gdn_ng_d.ap().partition_broadcast(128), "st7")
    dma("sp", rowc[:, 144:208], r_bias_d.ap().partition_broadcast(128), "st8")
    dgcol = CA.a("dgcol", [128, 2], F32)
    dma("sp", dgcol[:, 0:1], dil_qg_d.ap().rearrange("(p o) -> p o", o=1), "st9")
    dma("sp", dgcol[:, 1:2], dil_kg_d.ap().rearrange("(p o) -> p o", o=1), "st10")
    ngcol = CA.a("ngcol", [128, 1], F32)
    dma("sp", ngcol[:, 0:1], gdn_ng_d.ap().rearrange("(p o) -> p o", o=1), "st11")

    hT = S.sb("hT", [128, 8, T], BF, off=B0 + 16 * KB)
    y_aT = S.sb("y_aT", [128, 8, T], BF, off=B0 + 48 * KB)
    y_bT = S.sb("y_bT", [128, 4, T], BF, off=B0 + 80 * KB)
    mod = CA.a("mod", [128, 48], F32)
    st1 = CA.a("st1", [128, 8], F32)
    wada = [WA.a("wada%d" % i, [128, 8, 512], BF) for i in range(2)]
    xt = [WA.a("xt%d" % i, [128, D], F32) for i in range(2)]
    xn = [WA.a("xn%d" % i, [128, D], F32) for i in range(2)]
    junk = WA.a("junk", [128, D], BF)
    xnT = WA.a("xnT", [128, 8, T], F32)
    pm = bacq()

    def x_tile(t):
        i = t % 2
        dma("sp", xt[i][:], x_d.ap()[t * 128:(t + 1) * 128, :], "xt%d" % i)
        ss = st1[:, 0:1]
        act(junk[:], xt[i][:], AF.Square, accum=ss)
        act(st1[:, 1:2], ss, AF.Sqrt, bias=EPS, scale=1.0 / D)
        recip(st1[:, 2:3], st1[:, 1:2])
        ts(xn[i][:], xt[i][:], st1[:, 2:3])
        for half in range(2):
            pt = bank()
            for q in range(4):
                kc = half * 4 + q
                tr(pt[:, q * 128:(q + 1) * 128], xn[i][:, kc * 128:(kc + 1) * 128], ident[:])
            cp(xnT[:, half * 4:(half + 1) * 4, t * 128:(t + 1) * 128],
               pt[:, :].rearrange("p (a b) -> p a b", b=128), eng=("act" if half == 0 else "dve"))

    tiles_at = [2, 1, 1, 2, 1, 1, 2, 1, 1, 2, 1, 1]
    tnext = 0
    for blk in range(12):
        wt = wada[blk % 2]
        dma("pool", wt[:], w_ada_d.ap()[:, blk * 512:(blk + 1) * 512].rearrange("(k p) n -> p k n", p=128), "wada%d" % (blk % 2))
        if blk >= 1:
            for _ in range(tiles_at[blk - 1]):
                x_tile(tnext)
                tnext += 1
        for j in range(4):
            col = blk * 4 + j
            for kc in range(8):
                mm(pm[:, col:col + 1], wt[:, kc, j * 128:(j + 1) * 128], cs_b[:, kc:kc + 1], start=(kc == 0), stop=(kc == 7))
    while tnext < NT:
        x_tile(tnext)
        tnext += 1
    tt(mod[:], pm[:, 0:48], cols[:, 24:72], ALU.add)
    brel(pm)
    AB = CA.a("AB", [128, 32], F32)
    stt(AB[:, 0:8], mod[:, 8:16], 1.0, cols[:, 8:16], ALU.add, ALU.mult)
    cp(AB[:, 8:16], mod[:, 0:8])
    stt(AB[:, 16:24], mod[:, 32:40], 1.0, cols[:, 16:24], ALU.add, ALU.mult)
    cp(AB[:, 24:32], mod[:, 24:32])
    for kc in range(8):
        if kc % 2 == 0:
            act(hT[:, kc, :], xnT[:, kc, :], AF.Identity, bias=AB[:, 8 + kc:9 + kc], scale=AB[:, kc:kc + 1])
        else:
            ts(hT[:, kc, :], xnT[:, kc, :], AB[:, kc:kc + 1], AB[:, 8 + kc:9 + kc], ALU.mult, ALU.add)
    gtB = CA.a("gtB", [128, 2, 1024], F32)
    dg = WA.a("dg", [128, 4, 128], F32)
    for gi, c0 in enumerate((16, 40)):
        for half in range(2):
            for q in range(4):
                kc = half * 4 + q
                ts(dg[:, q, :], ident[:], mod[:, c0 + kc:c0 + kc + 1])
            pg = bank()
            mm(pg[:, :], ones_f[:], dg[:].rearrange("p a b -> p (a b)"))
            cp(gtB[:, gi, half * 512:(half + 1) * 512], pg[:, :], eng="act")

    if "mod" in dbg:
        dump("mod", mod[:], [128, 48])
        dump("gtB", gtB[:], [128, 2, 1024])

    if "hT" in dbg:
        dump("hT", hT[:], [128, 8, T])
    if stage <= 1:
        return finish()

    S.barrier()
    WA.cur = WA.base
    wba = WA.a("wba", [128, 8, 16], BF)
    dma("pool", wba[:], w_in_d.ap()[:, OFF_GB:OFF_GB + 16].rearrange("(k p) n -> p k n", p=128), "wba")
    ba = WA.a("ba", [128, 16, 16], F32)
    pbk = bank()
    for t in range(NT):
        for kc in range(8):
            mm(pbk[:, t * 16:(t + 1) * 16], hT[:, kc, t * 128:(t + 1) * 128], wba[:, kc, :], start=(kc == 0), stop=(kc == 7))
    cp(ba[:], pbk[:, 0:256].rearrange("p (a b) -> p a b", b=16))
    NS = lambda nm: WA.a(nm, [128, 16, 8], F32)
    beta, lb, gg, gc, gcl, egc, edec, bg, egt0, egt1, tmp8 = [NS(n) for n in
        ("beta", "lb", "gg", "gc", "gcl", "egc", "edec", "bg", "egt0", "egt1", "tmp8")]
    nea = WA.a("nea", [128, 8], F32)
    act(beta[:], ba[:, :, 0:8], AF.Sigmoid)
    act(lb[:], beta[:], AF.Ln)
    tt(tmp8[:], ba[:, :, 8:16], rowc[:, None, 8:16].to_broadcast([128, 16, 8]), ALU.add)
    act(tmp8[:], tmp8[:], AF.Exp)
    act(tmp8[:], tmp8[:], AF.Ln, bias=1.0)
    act(nea[:], rowc[:, 0:8], AF.Exp)
    ts(nea[:], nea[:], -1.0)
    tt(gg[:], tmp8[:], nea[:, None, :].to_broadcast([128, 16, 8]), ALU.mult)
    pg = bank()
    g2 = gg[:].rearrange("p a b -> p (a b)")
    mm(pg[:, 0:128], tri2[:], g2)
    mm(pg[:, 128:256], blk2[:], g2)
    mm(pg[:, 256:384], half01[:, 0, :], g2)
    mm(pg[:, 384:512], half01[:, 1, :], g2)
    v3 = lambda ap: ap.rearrange("p (a b) -> p a b", b=8)
    cp(gc[:], v3(pg[:, 0:128]))
    tt(gcl[:], gc[:], lb[:], ALU.add)
    act(egc[:], gc[:], AF.Exp)
    tt(tmp8[:], v3(pg[:, 128:256]), gc[:], ALU.subtract)
    act(edec[:], tmp8[:], AF.Exp)
    tt(bg[:], beta[:], egc[:], ALU.mult)
    act(egt0[:], v3(pg[:, 256:384]), AF.Exp)
    act(egt1[:], v3(pg[:, 384:512]), AF.Exp)
    egt = (egt0, egt1)
    ngB = rowc[:, 16:144]

    if "gsc" in dbg:
        dump("beta", beta[:], [128, 16, 8])
        dump("gc", gc[:], [128, 16, 8])
        dump("egt0", egt0[:], [128, 16, 8])

    def so_set(i):
        d = {}
        d["qT"] = WA.a("so_qT%d" % i, [128, T], BF)
        d["wT"] = WA.a("so_wT%d" % i, [128, T], BF)
        d["u"] = WA.a("so_u%d" % i, [128, 16, 128], BF)
        d["qkT"] = WA.a("so_qkT%d" % i, [128, 16, 128], BF)
        d["kdec"] = WA.a("so_kdec%d" % i, [128, 16, 128], BF)
        d["zg"] = WA.a("so_zg%d" % i, [128, T], BF)
        d["nw"] = WA.a("so_nw%d" % i, [128, 16, 128], BF)
        return d

    SO = [so_set(0), so_set(1)]
    w4 = WA.a("w4", [128, 8, 4, 128], BF)
    xc = WA.a("xc", [128, 2048 + 16], BF)
    kT = WA.a("kT", [128, T], BF)
    vs = WA.a("vs", [128, T], BF)
    ktw = WA.a("ktw", [128, 16, 128], BF)
    vb = WA.a("vb", [128, 16, 128], BF)
    qs = WA.a("qs", [128, 512], F32)
    sq = WA.a("sq", [128, 512], BF)
    rt = WA.a("rt", [128, 512], F32)
    dgc = WA.a("dgc", [128, 4, 128], BF)
    NTT = 6
    for a_ in dbg:
        if a_.startswith("ntt"):
            NTT = int(a_[3:])
    TA = Arena(B0 + 80 * KB, B0 + 96 * KB)
    TMP = []
    for i in range(NTT):
        d = {}
        AR = TA if i >= 3 else WA
        d["X"] = AR.a("tX%d" % i, [128, 2, 128], F32)
        d["Dm"] = d["X"]
        d["E"] = d["X"]
        d["B"] = [AR.a("tB%d_%d" % (i, k), [128, 3, 128], F32) for k in range(2)]
        d["Gb"] = (WA if i == 5 else AR).a("tGb%d" % i, [128, 128], BF)
        TMP.append(d)
    Sf = TA.a("Sf", [128, 128], F32)
    negm = WA.a("negm", [128, 2, 128], F32)
    ts(negm[:], mask2[:], 1.0, 30000.0, ALU.subtract, ALU.mult)
    Sb2 = [TA.a("Sb2_%d" % i, [128, 128], BF) for i in range(2)]
    LA = 2
    RING = LA + 1
    MTr = [TA.a("MTr%d" % i, [128, 128], BF) for i in range(RING)]
    vn = TA.a("vn", [128, 128], BF)
    ot = [TA.a("ot%d" % i, [128, 128], F32) for i in range(2)]
    yt = TA.a("yt", [128, 128], BF)
    junkg = TA.a("junkg", [128, 128], BF)
    memset(xc[:, 0:3], 0.0)
    if "mem" in dbg:
        print("GDN WA used", (WA.cur - WA.base) / 1024, "of", (WA.limit - WA.base) / 1024, "TA used", (TA.cur - TA.base) / 1024)

    def gdn_tile(h, t, so, tm):
        X, Dm, E, B = tm["X"], tm["Dm"], tm["E"], tm["B"]
        tsl = slice(t * 128, (t + 1) * 128)
        ts(X[:, 0, :], ident[:], gc[:, t, h:h + 1])
        ts(X[:, 1, :], ident[:], gcl[:, t, h:h + 1])
        yield
        pR = bacq()
        mm(pR[:, 0:256], ones_f[:], X[:].rearrange("p a b -> p (a b)"))
        mm(pR[:, 256:384], kT[:, tsl], so["qT"][:, tsl])
        mm(pR[:, 384:512], kT[:, tsl], kT[:, tsl])
        yield
        stt(Dm[:].rearrange("p a b -> p (a b)"), pR[:, 0:256], gc[:, t, h:h + 1],
            negm[:].rearrange("p a b -> p (a b)"), ALU.subtract, ALU.add)
        yield
        act(E[:], Dm[:], AF.Exp)
        yield
        tt(so["qkT"][:, t, :], pR[:, 256:384], E[:, 0, :], ALU.mult)
        tt(B[0][:, 0, :], pR[:, 384:512], E[:, 1, :], ALU.mult)
        brel(pR)
        yield
        pT = bacq()
        tr(pT[:, 0:128], B[0][:, 0, :], ident[:])
        yield
        cp(B[0][:, 2, :], pT[:, 0:128], eng="act")
        tt(B[1][:, 1, :], ident[:], B[0][:, 0, :], ALU.subtract, eng="pool")
        brel(pT)
        yield
        pa = bacq()
        mm(pa[:, 0:128], B[0][:, 2, :], B[0][:, 0, :])
        mm(pa[:, 256:384], B[0][:, 0, :], B[0][:, 2, :])
        yield
        pav = pa[:, 0:384].rearrange("p (a b) -> p a b", b=128)
        cp(B[1][:, 0:3:2, :], pav[:, 0:3:2, :], eng="act")
        brel(pa)
        yield
        cur = 1
        for k in range(1, 5):
            nxt = 1 - cur
            pa = bacq()
            mm(pa[:, 0:256], B[cur][:, 2, :], B[cur][:, 0:2, :].rearrange("p a b -> p (a b)"))
            mm(pa[:, 256:384], B[cur][:, 0, :], B[cur][:, 2, :])
            yield
            pav = pa[:, 0:384].rearrange("p (a b) -> p a b", b=128)
            cp(B[nxt][:, 0:3:2, :], pav[:, 0:3:2, :], eng="act")
            tt(B[nxt][:, 1, :], B[cur][:, 1, :], pa[:, 128:256], ALU.add)
            brel(pa)
            cur = nxt
            yield
        pa = bacq()
        mm(pa[:, 0:128], B[cur][:, 2, :], B[cur][:, 1, :])
        yield
        G = tm["Gb"][:, :]
        tt(G, B[cur][:, 1, :], pa[:, 0:128], ALU.add)
        brel(pa)
        yield
        pu = bacq()
        mm(pu[:, 0:128], G, vb[:, t, :])
        mm(pu[:, 128:256], ktw[:, t, :], G)
        mm(pu[:, 256:384], G, ktw[:, t, :])
        yield
        cp(so["u"][:, t, :], pu[:, 0:128], eng="act")
        cp(so["wT"][:, tsl], pu[:, 128:256])
        ts(so["nw"][:, t, :], pu[:, 256:384], -1.0)
        brel(pu)
        yield

    def w4_load(h):
        for j, off in enumerate((OFF_GQ, OFF_GK, OFF_GV, OFF_GZ)):
            dma("pool", w4[:, :, j, :], w_in_d.ap()[:, off + h * 128: off + (h + 1) * 128].rearrange("(k p) n -> p k n", p=128), "w4_%d" % j)

    def gdn_pre(h, so):
        for jj in range(3):
            ct = jj * 8 + h
            for tap in range(4):
                ts(dgc[:, tap, :], ident[:], convcol[:, tap * 24 + ct: tap * 24 + ct + 1])
            for tb in range(4):
                pb = bank()
                for kc in range(8):
                    mm(pb[:, :], w4[:, kc, jj, :], hT[:, kc, tb * 512:(tb + 1) * 512], start=(kc == 0), stop=(kc == 7))
                cp(xc[:, 3 + tb * 512: 3 + (tb + 1) * 512], pb[:, :], eng="act")
                if tb % 2 == 1:
                    yield
            for tb in range(4):
                pb = bank()
                for tap in range(4):
                    mm(pb[:, :], dgc[:, tap, :], xc[:, tb * 512 + tap: tb * 512 + tap + 512], start=(tap == 0), stop=(tap == 3))
                bsl = slice(tb * 512, (tb + 1) * 512)
                if jj == 2:
                    act(vs[:, bsl], pb[:, :], AF.Silu)
                else:
                    act(qs[:], pb[:, :], AF.Silu)
                    act(sq[:], qs[:], AF.Square)
                    p2 = bank()
                    mm(p2[:, :], ones_b[:], sq[:])
                    act(rt[:], p2[:, :], AF.Sqrt, bias=EPS)
                    recip(rt[:], rt[:])
                    if jj == 0:
                        stt(so["qT"][:, bsl], qs[:], float(128 ** -0.5), rt[:], ALU.mult, ALU.mult)
                    else:
                        tt(kT[:, bsl], qs[:], rt[:], ALU.mult)
                if tb % 2 == 1:
                    yield
        for tb in range(4):
            pb = bank()
            for kc in range(8):
                mm(pb[:, :], w4[:, kc, 3, :], hT[:, kc, tb * 512:(tb + 1) * 512], start=(kc == 0), stop=(kc == 7))
            act(qs[:], pb[:, :], AF.Silu)
            ts(so["zg"][:, tb * 512:(tb + 1) * 512], qs[:], ngcol[:, 0:1])
            if tb % 2 == 1:
                yield
        if h + 1 < NH:
            w4_load(h + 1)
        yield
        for g4 in range(4):
            pb = bank()
            pv = bf(pb[:, :])
            for q in range(4):
                t = g4 * 4 + q
                tr(pv[:, q * 128:(q + 1) * 128], kT[:, t * 128:(t + 1) * 128], identb[:])
                tr(pv[:, 512 + q * 128: 512 + (q + 1) * 128], vs[:, t * 128:(t + 1) * 128], identb[:])
            kv_ = pv[:, 0:512].rearrange("p (a b) -> p a b", b=128)
            vv_ = pv[:, 512:1024].rearrange("p (a b) -> p a b", b=128)
            g4s = slice(g4 * 4, (g4 + 1) * 4)
            tt(ktw[:, g4s, :], kv_, bg[:, g4s, h:h + 1].to_broadcast([128, 4, 128]), ALU.mult)
            tt(so["kdec"][:, g4s, :], kv_, edec[:, g4s, h:h + 1].to_broadcast([128, 4, 128]), ALU.mult)
            tt(vb[:, g4s, :], vv_, beta[:, g4s, h:h + 1].to_broadcast([128, 4, 128]), ALU.mult)
            yield
        pend = list(range(NT if "notile" not in dbg else 0))
        active = []
        free_tmp = list(range(NTT))
        while pend or active:
            if pend and free_tmp:
                ti = free_tmp.pop(0)
                active.append((gdn_tile(h, pend.pop(0), so, TMP[ti]), ti))
            nxt = []
            for g, ti in active:
                try:
                    next(g)
                    nxt.append((g, ti))
                except StopIteration:
                    free_tmp.append(ti)
            active = nxt
            yield

    def gdn_scan(h, so):
        memset(Sf[:], 0.0)
        memset(Sb2[0][:], 0.0)
        NS_ = 32

        def rng(n):
            t, half = divmod(n, 2)
            lo, hi = half * 64, half * 64 + 64
            return t, half, lo, hi

        for n0 in range(LA):
            t, half, lo, hi = rng(n0)
            pM = bacq()
            mm(pM[:, 0:128], so["nw"][lo:hi, t, :], so["kdec"][lo:hi, t, :])
            yield
            cp(MTr[n0 % RING][:], pM[:, 0:128], eng="act")
            brel(pM)
            yield
        later = {}

        def at(r, fn):
            later.setdefault(r, []).append(fn)

        pY = [None]

        def getY():
            if pY[0] is None:
                pY[0] = bacq()
            return pY[0]

        def fin_a(t):
            o_ = ot[t % 2]
            act(junkg[:], o_[:], AF.Square, accum=st1[:, 3:4])
            act(st1[:, 4:5], st1[:, 3:4], AF.Ln, bias=EPS, scale=1.0 / 128)
            act(st1[:, 5:6], st1[:, 4:5], AF.Exp, scale=-0.5)

        def fin_b(t):
            o_ = ot[t % 2]
            ts(yt[:], o_[:], st1[:, 5:6])

        def fin_c(t):
            py = getY()
            tr(bf(py[:, :])[:, 512:640], yt[:], identb[:])

        def fin_d(t):
            py = getY()
            tt(y_aT[:, h, t * 128:(t + 1) * 128], bf(py[:, :])[:, 512:640], so["zg"][:, t * 128:(t + 1) * 128], ALU.mult)

        prev = None
        r = 0
        n = 0
        while n < NS_ or prev is not None or any(k >= r for k in later):
            pX = None
            cur = None
            if n < NS_:
                t, half, lo, hi = rng(n)
                tok = slice(t * 128 + lo, t * 128 + hi)
                Sc = Sb2[n % 2]
                pX = bacq()
                mm(pX[:, 256:384], so["kdec"][lo:hi, t, :], so["u"][lo:hi, t, :], start=True, stop=False)
                mm(pX[:, 256:384], MTr[n % RING][:], Sc[:, :], start=False, stop=True)
                mm(pX[lo:hi, 0:128], so["wT"][:, tok], Sc[:, :])
                mm(pX[lo:hi, 128:256], so["qT"][:, tok], Sc[:, :])
                cur = (n, t, half, lo, hi)
            if prev is not None:
                pn, pt_, phalf, plo, phi = prev
                if pX is None:
                    pX = bacq()
                mm(pX[plo:phi, 384:512], so["qkT"][plo:phi, pt_, plo:phi], vn[plo:phi, :])
            nla = n + LA
            if nla < NS_:
                t2, half2, lo2, hi2 = rng(nla)
                py = getY()
                mm(py[:, 0:128], so["nw"][lo2:hi2, t2, :], so["kdec"][lo2:hi2, t2, :])
            for fn in later.pop(r, []):
                fn()
            r += 1
            yield
            if cur is not None:
                Sn = Sb2[(n + 1) % 2]
                stt(Sn[:], Sf[:], egt[half][:, t, h:h + 1], pX[:, 256:384], ALU.mult, ALU.add)
                stt(Sf[:], Sf[:], egt[half][:, t, h:h + 1], pX[:, 256:384], ALU.mult, ALU.add)
            if prev is not None:
                pn, pt_, phalf, plo, phi = prev
                po_ = ot[pt_ % 2]
                tt(po_[plo:phi, :], po_[plo:phi, :], pX[plo:phi, 384:512], ALU.add)
                if phalf == 1:
                    fin_a(pt_)
                    at(r + 2, lambda pt_=pt_: fin_b(pt_))
                    at(r + 3, lambda pt_=pt_: fin_c(pt_))
                    at(r + 4, lambda pt_=pt_: fin_d(pt_))
            if cur is not None:
                o_ = ot[t % 2]
                tt(vn[lo:hi, :], so["u"][lo:hi, t, :], pX[lo:hi, 0:128], ALU.subtract)
                act(o_[lo:hi, :], pX[lo:hi, 128:256], AF.Copy, scale=egc[lo:hi, t, h:h + 1])
            if pX is not None:
                brel(pX)
            if nla < NS_:
                cp(MTr[nla % RING][:], pY[0][:, 0:128], eng="act")
            for fn in later.pop(r, []):
                fn()
            if pY[0] is not None:
                brel(pY[0])
                pY[0] = None
            r += 1
            prev = cur
            n += 1
            yield

    def multi(g, k):
        while True:
            for _ in range(k):
                try:
                    next(g)
                except StopIteration:
                    return
            yield

    NH = 8 if "gdn1" not in dbg else 1
    if "gdn2" in dbg:
        NH = 2
    w4_load(0)
    run_threads([gdn_pre(0, SO[0])])
    for h in range(NH):
        SM = 1
        for a_ in dbg:
            if a_.startswith("sm"):
                SM = int(a_[2:])
        th = [multi(gdn_scan(h, SO[h % 2]), SM)]
        if "noil" in dbg:
            run_threads(th)
            th = []
        if h + 1 < NH:
            th.append(gdn_pre(h + 1, SO[(h + 1) % 2]))
        run_threads(th)

    if "gdn" in dbg:
        dump("y_aT", y_aT[:, 0:NH, :], [128, NH, T])
        sd = SO[(NH - 1) % 2]
        dump("so_qT", sd["qT"][:], [128, T])
        dump("kT", kT[:], [128, T])
        dump("so_u", sd["u"][:], [128, 16, 128])
        dump("so_wT", sd["wT"][:], [128, T])
        dump("so_qkT", sd["qkT"][:], [128, 16, 128])
        dump("so_kdec", sd["kdec"][:], [128, 16, 128])
        dump("ktw", ktw[:], [128, 16, 128])
        dump("vb", vb[:], [128, 16, 128])
        dump("bg", bg[:], [128, 16, 8])
    if stage <= 2:
        return finish()

    S.barrier()
    WA.cur = WA.base
    ts(dgcol[:, 0:1], dgcol[:, 0:1], float(128 ** -0.5))
    DB = []
    for i in range(2):
        d = {}
        d["w"] = WA.a("dw%d" % i, [128, 8, 3, 128], BF)
        d["q"] = WA.a("dq%d" % i, [128, T], BF)
        d["k"] = WA.a("dk%d" % i, [128, T], BF)
        d["vt"] = WA.a("dvt%d" % i, [128, 16, 128], BF)
        DB.append(d)
    dvT = WA.a("dvT", [128, T], BF)
    sqd = WA.a("sqd", [128, 512], BF)
    rtd = WA.a("rtd", [128, 512], F32)
    acc = WA.a("acc", [128, 2, T], F32)
    Pf = [WA.a("Pf%d" % i, [128, 2, 128], BF) for i in range(6)]
    PTb = [WA.a("PTb%d" % i, [128, 2, 128], BF) for i in range(6)]
    DILS = (1, 4, 16)

    def dil_w_load(n):
        j, g = divmod(n, 3)
        hd = g * 4 + j
        for jj, off in enumerate((OFF_DQ, OFF_DK, OFF_DV)):
            dma("pool", DB[n % 2]["w"][:, :, jj, :], w_in_d.ap()[:, off + hd * 128: off + (hd + 1) * 128].rearrange("(k p) n -> p k n", p=128), "dw%d_%d" % (n % 2, jj))

    def dil_pre(n, db):
        j, g = divmod(n, 3)
        hd = g * 4 + j
        dil = DILS[g]
        if n + 1 < NDH:
            dil_w_load(n + 1)
        mb = 512 // dil
        for jj in range(3):
            dst = (db["q"], db["k"], dvT)[jj]
            for tb in range(4):
                pb = bank()
                for kc in range(8):
                    mm(pb[:, :], db["w"][:, kc, jj, :], hT[:, kc, tb * 512:(tb + 1) * 512], start=(kc == 0), stop=(kc == 7))
                if dil == 1:
                    dview = dst[:, tb * 512:(tb + 1) * 512]
                    sview = pb[:, :]
                    rview = rtd[:, :]
                else:
                    dview = dst[:, :].rearrange("p (r m) -> p r m", r=dil)[:, :, tb * mb:(tb + 1) * mb]
                    sview = pb[:, :].rearrange("p (m r) -> p r m", r=dil)
                    rview = rtd[:, :].rearrange("p (m r) -> p r m", r=dil)
                if jj == 2:
                    cp(dview, sview, eng="act")
                else:
                    act(sqd[:], pb[:, :], AF.Square)
                    p2 = bank()
                    mm(p2[:, :], ones_b[:], sqd[:])
                    act(rtd[:], p2[:, :], AF.Ln, bias=EPS, scale=1.0 / 128)
                    act(rtd[:], rtd[:], AF.Exp, scale=-0.5)
                    stt(dview, sview, dgcol[:, jj:jj + 1], rview, ALU.mult, ALU.mult)
                if True:
                    yield
        for g4 in range(4):
            pb = bank()
            pv = bf(pb[:, :])
            for q in range(4):
                t = g4 * 4 + q
                tr(pv[:, q * 128:(q + 1) * 128], dvT[:, t * 128:(t + 1) * 128], identb[:])
            cp(db["vt"][:, g4 * 4:(g4 + 1) * 4, :], pv[:, 0:512].rearrange("p (a b) -> p a b", b=128), eng="act")
            yield

    NDT = 6

    def dil_qtile(n, db, tq, ti):
        j, g = divmod(n, 3)
        dil = DILS[g]
        ntr = 16 // dil
        r, mt = divmod(tq, ntr)
        qs_ = slice(tq * 128, (tq + 1) * 128)
        P_, PT_ = Pf[ti], PTb[ti]
        pS = bacq()
        if mt > 0:
            mm(pS[:, 0:128], db["k"][:, (tq - 1) * 128: tq * 128], db["q"][:, qs_])
        mm(pS[:, 128:256], db["k"][:, qs_], db["q"][:, qs_])
        yield
        if mt > 0:
            act(P_[:].rearrange("p a b -> p (a b)"), pS[:, 0:256], AF.Exp)
        else:
            act(P_[:, 1, :], pS[:, 128:256], AF.Exp)
        brel(pS)
        yield
        if mt > 0:
            tt(PT_[:], P_[:], amask[:], ALU.mult, eng="pool")
        else:
            tt(PT_[:, 1, :], P_[:, 1, :], amask[:, 1, :], ALU.mult, eng="pool")
        yield
        pN = bacq()
        if mt > 0:
            mm(pN[:, 0:128], db["vt"][:, tq - 1, :], PT_[:, 0, :], start=True, stop=False)
        mm(pN[:, 0:128], db["vt"][:, tq, :], PT_[:, 1, :], start=(mt == 0), stop=True)
        if mt > 0:
            mm(pN[:, 128:256], ones_b[:], PT_[:, 0, :], start=True, stop=False)
        mm(pN[:, 128:256], ones_b[:], PT_[:, 1, :], start=(mt == 0), stop=True)
        yield
        st = r + mt * 128 * dil
        aview = acc[:, :, st: st + 127 * dil + 1: dil]
        nview = pN[:, 0:256].rearrange("p (a b) -> p a b", b=128)
        if g == 0:
            cp(aview, nview, eng="act")
        else:
            tt(aview, aview, nview, ALU.add)
        brel(pN)
        yield

    def dil_att(n, db):
        j, g = divmod(n, 3)
        pend = list(range(16))
        active = []
        free_t = list(range(NDT))
        while pend or active:
            if pend and free_t:
                ti = free_t.pop(0)
                active.append((dil_qtile(n, db, pend.pop(0), ti), ti))
            nxt = []
            for gq, ti in active:
                try:
                    next(gq)
                    nxt.append((gq, ti))
                except StopIteration:
                    free_t.append(ti)
            active = nxt
            yield
        if g == 2:
            recip(acc[:, 1, :], acc[:, 1, :])
            tt(y_bT[:, j, :], acc[:, 0, :], acc[:, 1, :], ALU.mult)
            yield

    NDH = 12 if "dil1" not in dbg else 3
    dil_w_load(0)
    run_threads([dil_pre(0, DB[0])])
    for n in range(NDH):
        th = [dil_att(n, DB[n % 2])]
        if n + 1 < NDH:
            th.append(dil_pre(n + 1, DB[(n + 1) % 2]))
        run_threads(th)
    if "dil" in dbg:
        dump("y_bT", y_bT[:, 0:NDH // 3, :], [128, NDH // 3, T])
    if stage <= 3:
        return finish()

    S.barrier()
    WA.cur = WA.base
    mergedT = WA.a("mergedT", [128, 8, T], BF)
    wg2 = [WA.a("wg2_%d" % i, [128, 8, 2, 128], BF) for i in range(2)]
    wupa = [WA.a("wupa%d" % i, [128, 8, 128], BF) for i in range(2)]
    wupd = [WA.a("wupd%d" % i, [128, 4, 128], BF) for i in range(2)]
    sga = [WA.a("sga%d" % i, [128, 512], F32) for i in range(2)]
    sgb = [WA.a("sgb%d" % i, [128, 512], F32) for i in range(2)]
    m1 = [WA.a("m1_%d" % i, [128, 512], BF) for i in range(2)]
    m2 = [WA.a("m2_%d" % i, [128, 512], BF) for i in range(2)]
    wo = WA.a("wo", [128, 8, D], BF)
    xt2 = [WA.a("xt2_%d" % i, [128, D], F32) for i in range(2)]
    dma("pool", wo[:], w_out_d.ap().rearrange("(k p) n -> p k n", p=128), "wo")
    tt(wo[:], wo[:], gtB[:, 0:1, :].to_broadcast([128, 8, D]), ALU.mult, eng="pool")
    for ft in range(8):
        i = ft % 2
        fsl = slice(ft * 128, (ft + 1) * 128)
        dma("pool", wg2[i][:, :, 0, :], w_in_d.ap()[:, OFF_GA_GATE + ft * 128: OFF_GA_GATE + (ft + 1) * 128].rearrange("(k p) n -> p k n", p=128), "wg2a%d" % i)
        dma("pool", wg2[i][:, :, 1, :], w_in_d.ap()[:, OFF_GB_GATE + ft * 128: OFF_GB_GATE + (ft + 1) * 128].rearrange("(k p) n -> p k n", p=128), "wg2b%d" % i)
        dma("pool", wupa[i][:], w_upg_d.ap()[:, fsl].rearrange("(k p) n -> p k n", p=128), "wupa%d" % i)
        dma("pool", wupd[i][:], w_upd_d.ap()[:, fsl].rearrange("(k p) n -> p k n", p=128), "wupd%d" % i)
        for tb in range(4):
            k2 = tb % 2
            bsl = slice(tb * 512, (tb + 1) * 512)
            pa, pb, pc, pd = bank(), bank(), bank(), bank()
            for kc in range(8):
                mm(pa[:, :], wg2[i][:, kc, 0, :], hT[:, kc, bsl], start=(kc == 0), stop=(kc == 7))
            for kc in range(8):
                mm(pb[:, :], wg2[i][:, kc, 1, :], hT[:, kc, bsl], start=(kc == 0), stop=(kc == 7))
            for hc in range(8):
                mm(pc[:, :], wupa[i][:, hc, :], y_aT[:, hc, bsl], start=(hc == 0), stop=(hc == 7))
            for sc_ in range(4):
                mm(pd[:, :], wupd[i][:, sc_, :], y_bT[:, sc_, bsl], start=(sc_ == 0), stop=(sc_ == 3))
            act(sga[k2][:], pa[:, :], AF.Sigmoid)
            act(sgb[k2][:], pb[:, :], AF.Sigmoid)
            tt(m1[k2][:], sga[k2][:], pc[:, :], ALU.mult)
            tt(m2[k2][:], sgb[k2][:], pd[:, :], ALU.mult)
            tt(mergedT[:, ft, bsl], m1[k2][:], m2[k2][:], ALU.add, eng="pool")
    if "mrg" in dbg:
        dump("mergedT", mergedT[:], [128, 8, T])
    S.barrier()
    x1 = S.sb("x1", [128, 16, D], F32, off=B0 + 16 * KB)
    for t in range(NT):
        i = t % 2
        dma("sp", xt2[i][:], x_d.ap()[t * 128:(t + 1) * 128, :], "xt2_%d" % i)
        for half in range(2):
            hs = slice(half * 512, (half + 1) * 512)
            pb = bank()
            for ft in range(8):
                mm(pb[:, :], mergedT[:, ft, t * 128:(t + 1) * 128], wo[:, ft, hs], start=(ft == 0), stop=(ft == 7))
            tt(x1[:, t, hs], xt2[i][:, hs], pb[:, :], ALU.add)
    if "x1" in dbg:
        dump("x1", x1[:], [128, 16, D])
    if stage <= 4:
        return finish()

    S.barrier()
    MA = Arena(B0 + 80 * KB, B0 + 207 * KB)
    h2T = MA.a("h2T", [128, 8, T], BF)
    cT = MA.a("cT", [64, T], BF)
    wgu = [MA.a("wgu%d" % i, [128, 8, 512], BF) for i in range(3)]
    wdn = [MA.a("wdn%d" % i, [128, 2, D], BF) for i in range(6)]
    sact = [MA.a("sact%d" % i, [128, 512], BF) for i in range(2)]
    tmid = [MA.a("tmid%d" % i, [128, 512], BF) for i in range(2)]
    sel_e = [MA.a("sel_e%d" % i, [64, 128], BF) for i in range(2)]
    mark = MA.cur
    hTe = [[MA.a("hTe%d_%d" % (a, b), [128, 2, T], BF) for b in range(2)] for a in range(2)]
    MA.cur = mark
    wr = MA.a("wr", [128, 8, 64], F32)
    dma("sp", wr[:], w_r_d.ap().rearrange("(k p) n -> p k n", p=128), "wr")
    rbias = rowc[:, 144:208]
    NRT = 3
    RT = []
    for i in range(NRT):
        d = {}
        d["xn"] = MA.a("xn2_%d" % i, [128, D], F32)
        d["junk"] = MA.a("junk2_%d" % i, [128, D], BF)
        d["h2f"] = MA.a("h2f_%d" % i, [128, 8, 128], F32)
        for nm in ("scr", "sel", "selm", "wkk", "comb", "emk"):
            d[nm] = MA.a("%s_%d" % (nm, i), [128, 64], F32)
        d["top"] = MA.a("top_%d" % i, [128, 8, 8], F32)
        for nm in ("gs", "gtop", "gm", "t30", "top8", "st"):
            d[nm] = MA.a("%s_%d" % (nm, i), [128, 8], F32)
        RT.append(d)

    def router_tile(t, d):
        src_ap = x1[:, t, :]
        st_ = d["st"]
        xn_, h2f = d["xn"], d["h2f"]
        act(d["junk"][:], src_ap, AF.Square, accum=st_[:, 0:1])
        yield
        act(st_[:, 1:2], st_[:, 0:1], AF.Sqrt, bias=EPS, scale=1.0 / D)
        yield
        recip(st_[:, 2:3], st_[:, 1:2])
        yield
        ts(xn_[:], src_ap, st_[:, 2:3])
        yield
        for half in range(2):
            pt = bacq()
            for q in range(4):
                kc = half * 4 + q
                tr(pt[:, q * 128:(q + 1) * 128], xn_[:, kc * 128:(kc + 1) * 128], ident[:])
            yield
            for q in range(4):
                kc = half * 4 + q
                if q % 2 == 0:
                    act(h2f[:, kc, :], pt[:, q * 128:(q + 1) * 128], AF.Identity,
                        bias=AB[:, 24 + kc:25 + kc], scale=AB[:, 16 + kc:17 + kc])
                else:
                    ts(h2f[:, kc, :], pt[:, q * 128:(q + 1) * 128], AB[:, 16 + kc:17 + kc],
                       AB[:, 24 + kc:25 + kc], ALU.mult, ALU.add)
            brel(pt)
            yield
            cp(h2T[:, half * 4:(half + 1) * 4, t * 128:(t + 1) * 128], h2f[:, half * 4:(half + 1) * 4, :], eng="pool")
        pl = bacq()
        for kc in range(8):
            mm(pl[:, 0:64], h2f[:, kc, :], wr[:, kc, :], start=(kc == 0), stop=(kc == 7))
        yield
        scr, sel, selm, wkk, comb, emk = [d[n] for n in ("scr", "sel", "selm", "wkk", "comb", "emk")]
        top, gs, gtop, gm, t30, top8 = [d[n] for n in ("top", "gs", "gtop", "gm", "t30", "top8")]
        act(scr[:], pl[:, 0:64], AF.Sigmoid)
        brel(pl)
        yield
        tt(sel[:], scr[:], rbias, ALU.add)
        yield
        for g in range(8):
            S.add("dve", lambda e, g=g: e.max(out=top[:, g, :], in_=sel[:, g * 8:(g + 1) * 8]),
                  reads=[sel[:, g * 8:(g + 1) * 8]], writes=[top[:, g, :]])
        yield
        tt(gs[:], top[:, :, 0], top[:, :, 1], ALU.add)
        yield
        S.add("dve", lambda e: e.max(out=gtop[:], in_=gs[:]), reads=[gs[:]], writes=[gtop[:]])
        yield
        ts(gm[:], gs[:], gtop[:, 3:4], None, ALU.is_ge)
        yield
        ts(t30[:], gm[:], 30.0, -30.0, ALU.mult, ALU.add)
        sel3 = sel[:].rearrange("p (a b) -> p a b", b=8)
        selm3 = selm[:].rearrange("p (a b) -> p a b", b=8)
        tt(selm3, sel3, gm[:, :, None].to_broadcast([128, 8, 8]), ALU.mult)
        yield
        tt(selm3, selm3, t30[:, :, None].to_broadcast([128, 8, 8]), ALU.add)
        yield
        S.add("dve", lambda e: e.max(out=top8[:], in_=selm[:]), reads=[selm[:]], writes=[top8[:]])
        yield
        ts(emk[:], selm[:], top8[:, 7:8], None, ALU.is_ge)
        yield
        tt(wkk[:], scr[:], emk[:], ALU.mult)
        yield
        S.add("dve", lambda e: e.reduce_sum(out=st_[:, 3:4], in_=wkk[:], axis=AX.X), reads=[wkk[:]], writes=[st_[:, 3:4]])
        yield
        recip(st_[:, 4:5], st_[:, 3:4])
        yield
        ts(comb[:], wkk[:], st_[:, 4:5], 2.5, ALU.mult, ALU.mult)
        yield
        pc = bacq()
        tr(pc[0:64, 0:128], comb[:], ident[:])
        yield
        cp(cT[:, t * 128:(t + 1) * 128], pc[0:64, 0:128], eng="act")
        brel(pc)
        yield

    groups = [(2 * g, 2 * g + 1) for g in range(32)] + [(64,)]
    if "moe1" in dbg:
        groups = groups[:2] + [(64,)]
    elist = [e for grp in groups for e in grp]

    def load_w(e, slot):
        wb = wgu[slot % 3]
        wd_ = wdn[slot % 6]
        if e < 64:
            dma("pool", wb[:, :, 0:256], w_eg_d.ap()[e].rearrange("(k p) n -> p k n", p=128), "wgu_g%d" % (slot % 3))
            dma("pool", wb[:, :, 256:512], w_eu_d.ap()[e].rearrange("(k p) n -> p k n", p=128), "wgu_u%d" % (slot % 3))
            dma("pool", wd_[:], w_ed_d.ap()[e].rearrange("(k p) n -> p k n", p=128), "wdn%d" % (slot % 6))
        else:
            dma("pool", wb[:, :, 0:256], w_sg_d.ap().rearrange("(k p) n -> p k n", p=128), "wgu_g%d" % (slot % 3))
            dma("pool", wb[:, :, 256:512], w_su_d.ap().rearrange("(k p) n -> p k n", p=128), "wgu_u%d" % (slot % 3))
            dma("pool", wd_[:], w_sd_d.ap().rearrange("(k p) n -> p k n", p=128), "wdn%d" % (slot % 6))

    load_w(elist[0], 0)
    load_w(elist[1], 1)

    pend_t = list(range(NT))
    active = []
    free_r = list(range(NRT))
    while pend_t or active:
        if pend_t and free_r:
            ri = free_r.pop(0)
            active.append((router_tile(pend_t.pop(0), RT[ri]), ri))
        nxt_ = []
        for g_, ri in active:
            try:
                next(g_)
                nxt_.append((g_, ri))
            except StopIteration:
                free_r.append(ri)
        active = nxt_
    if "rt" in dbg:
        dump("h2T", h2T[:], [128, 8, T])
        dump("cT", cT[:], [64, T])
    if stage <= 5:
        return finish()

    S.barrier()


    def prep_w(e, slot):
        wd_ = wdn[slot % 6]
        tt(wd_[:], wd_[:], gtB[:, 1:2, :].to_broadcast([128, 2, D]), ALU.mult)
        if e < 64:
            ts(sel_e[slot % 2][:], ones_b[0:64, :], ident[0:64, e:e + 1])

    def gateup(e, slot, gi):
        wb = wgu[slot % 3]
        wd_ = wdn[slot % 6]
        if slot + 2 < len(elist):
            load_w(elist[slot + 2], slot + 2)
        he = hTe[gi % 2][slot % 2]
        se = sel_e[slot % 2]
        for tb in range(4):
            if tb == 1 and slot + 1 < len(elist):
                prep_w(elist[slot + 1], slot + 1)
            bsl = slice(tb * 512, (tb + 1) * 512)
            if e < 64:
                pcb = bank()
                mm(pcb[:, :], se[:], cT[:, bsl])
            for ft in range(2):
                k2 = (tb * 2 + ft) % 2
                pa, pu = bank(), bank()
                for kc in range(8):
                    mm(pa[:, :], wb[:, kc, ft * 128:(ft + 1) * 128], h2T[:, kc, bsl], start=(kc == 0), stop=(kc == 7))
                for kc in range(8):
                    mm(pu[:, :], wb[:, kc, 256 + ft * 128: 256 + (ft + 1) * 128], h2T[:, kc, bsl], start=(kc == 0), stop=(kc == 7))
                act(sact[k2][:], pa[:, :], AF.Silu)
                if e < 64:
                    tt(tmid[k2][:], sact[k2][:], pu[:, :], ALU.mult)
                    tt(he[:, ft, bsl], tmid[k2][:], pcb[:, :], ALU.mult)
                else:
                    tt(he[:, ft, bsl], sact[k2][:], pu[:, :], ALU.mult)
        return he, wd_

    def down(items, last):
        for t in range(NT):
            for half in range(2):
                hs = slice(half * 512, (half + 1) * 512)
                pb = bank()
                n = len(items) * 2
                k = 0
                for (he, wd_) in items:
                    for ft in range(2):
                        mm(pb[:, :], he[:, ft, t * 128:(t + 1) * 128], wd_[:, ft, hs], start=(k == 0), stop=(k == n - 1))
                        k += 1
                tt(x1[:, t, hs], x1[:, t, hs], pb[:, :], ALU.add)
            if last:
                finals.append(dma("sp", out_d.ap()[t * 128:(t + 1) * 128, :], x1[:, t, :], "out%d" % (t % 4)))

    prep_w(elist[0], 0)
    slot = 0
    prev = None
    for gi, grp in enumerate(groups):
        items = []
        for e in grp:
            items.append(gateup(e, slot, gi))
            slot += 1
        if prev is not None:
            down(prev, False)
        prev = items
    down(prev, True)
    return finish()


_IN_NAMES = ["x", "c", "w_ada", "b_ada", "g_mix", "w_in", "gdn_conv_w", "gdn_a_log", "gdn_dt_bias", "gdn_norm_g",
             "dil_q_norm_g", "dil_k_norm_g", "w_up_gdn", "w_up_dil", "w_out", "g_ffn", "w_router", "router_bias",
             "w_exp_gate", "w_exp_up", "w_exp_down", "w_sh_gate", "w_sh_up", "w_sh_down"]


def make_in_maps(inputs, n_cores=8):
    shared = {}
    for k in _IN_NAMES:
        if k in ("x", "c"):
            continue
        a = np.asarray(inputs[k], dtype=np.float32)
        shared[k] = np.ascontiguousarray(a[0])
    maps = []
    for b in range(n_cores):
        m = dict(shared)
        m["x"] = np.ascontiguousarray(np.asarray(inputs["x"], dtype=np.float32)[b])
        m["c"] = np.ascontiguousarray(np.asarray(inputs["c"], dtype=np.float32)[b])
        maps.append(m)
    return maps


def kernel(**inputs):
    nc, _ = build()
    maps = make_in_maps(inputs)
    res = run_bass_kernel_spmd(nc, maps, core_ids=list(range(8)))
    out = np.stack([np.asarray(r["out"], dtype=np.float32) for r in res.results], axis=0)
    return out
```

```python
import numpy as np
import concourse.bass as bass
import concourse.mybir as mybir
from concourse.bass_utils import run_bass_kernel_spmd

F32 = mybir.dt.float32
BF = mybir.dt.bfloat16
AF = mybir.ActivationFunctionType
ALU = mybir.AluOpType
AX = mybir.AxisListType

ENGS = ("pe", "act", "dve", "pool", "sp")

T = 2048
D = 1024
NT = 16
KC = 8
IN_W = 10768
EPS = 1e-6
N_EXP = 64
OFF_GQ, OFF_GK, OFF_GV, OFF_GZ, OFF_GB, OFF_GA = 0, 1024, 2048, 3072, 4096, 4104
OFF_DQ, OFF_DK, OFF_DV, OFF_GA_GATE, OFF_GB_GATE = 4112, 5648, 7184, 8720, 9744


class Sched:
    def __init__(self, nc):
        self.nc = nc
        self.eng = {"pe": nc.tensor, "act": nc.scalar, "dve": nc.vector,
                    "pool": nc.gpsimd, "sp": nc.sync}
        self.ops = {e: [] for e in ENGS}
        self.order = []
        self.fsz = {}
        self.kind = {}
        self.recs = {}
        self.dma_keys = {}
        self.dma_last = {}
        self.pending = {}

    def barrier(self):
        last = set()
        for e in ENGS:
            if self.ops[e]:
                last.add((e, len(self.ops[e]) - 1))
        for k, v in self.dma_last.items():
            last.add(v)
        for (de, di) in last:
            self.ops[de][di]["signal"] = True
        for e in ENGS:
            self.pending[e] = set(last) | self.pending.get(e, set())

    def sb(self, name, shape, dtype, off=None):
        if off is None:
            raise RuntimeError("explicit offset required")
        t = self.nc.alloc_sbuf_tensor_at(name, list(shape), dtype, offset=off)
        f = 1
        for s in shape[1:]:
            f *= s
        self.fsz[t.name] = f
        self.kind[t.name] = "sb"
        self.recs[t.name] = []
        return t

    def ps(self, name, shape, dtype):
        t = self.nc.alloc_psum_tensor(name, list(shape), dtype)
        f = 1
        for s in shape[1:]:
            f *= s
        self.fsz[t.name] = f
        self.kind[t.name] = "ps"
        self.recs[t.name] = []
        return t

    def box(self, ap):
        name = ap.tensor.name
        if name not in self.fsz:
            return None
        f = self.fsz[name]
        if self.kind[name] == "ps":
            return (name, 0, 128, 0, f)
        off = ap.offset
        dims = ap.ap
        plo = off // f
        phi = plo + dims[0][1]
        flo = off % f
        ext = 1
        for st, cnt in dims[1:]:
            ext += (cnt - 1) * abs(st)
        return (name, plo, phi, flo, flo + ext)

    def add(self, e, fn, reads=(), writes=(), dma_key=None):
        idx = len(self.ops[e])
        op = dict(fn=fn, deps=set(), dma_key=dma_key, signal=False, val=None)
        rb = [self.box(a) for a in reads]
        wb = [self.box(a) for a in writes]
        deps = set()
        for b in rb:
            if b is None:
                continue
            name, plo, phi, flo, fhi = b
            isps = self.kind[name] == "ps"
            for r in self.recs[name]:
                if (r[6] or isps) and r[0] < phi and plo < r[1] and r[2] < fhi and flo < r[3]:
                    deps.add((r[4], r[5]))
        for b in wb:
            if b is None:
                continue
            name, plo, phi, flo, fhi = b
            for r in self.recs[name]:
                if r[0] < phi and plo < r[1] and r[2] < fhi and flo < r[3]:
                    deps.add((r[4], r[5]))
        for b in wb:
            if b is None:
                continue
            name, plo, phi, flo, fhi = b
            lst = self.recs[name]
            lst[:] = [r for r in lst if not (plo <= r[0] and r[1] <= phi and flo <= r[2] and r[3] <= fhi)]
            lst.append([plo, phi, flo, fhi, e, idx, True])
        for b in rb:
            if b is None:
                continue
            name, plo, phi, flo, fhi = b
            lst = self.recs[name]
            if self.kind[name] == "ps":
                lst[:] = [[plo, phi, flo, fhi, e, idx, True]]
            else:
                lst[:] = [r for r in lst if not ((not r[6]) and r[4] == e and dma_key is None and r[5] < idx
                                                 and self.ops[e][r[5]]["dma_key"] is None
                                                 and plo <= r[0] and r[1] <= phi and flo <= r[2] and r[3] <= fhi)]
                lst.append([plo, phi, flo, fhi, e, idx, False])
        if e in self.pending:
            deps |= self.pending.pop(e)
        if dma_key is not None and dma_key in self.dma_last:
            deps.add(self.dma_last[dma_key])
        fdeps = set()
        best = {}
        for (de, di) in deps:
            dop = self.ops[de][di]
            if de == e and e == "pe" and dop["dma_key"] is None and dma_key is None:
                continue
            if dop["dma_key"] is not None:
                fdeps.add((de, di))
            elif best.get(de, -1) < di:
                best[de] = di
        for de, di in best.items():
            fdeps.add((de, di))
        for (de, di) in fdeps:
            self.ops[de][di]["signal"] = True
        op["deps"] = fdeps
        if dma_key is not None:
            op["signal"] = True
            if dma_key not in self.dma_keys:
                self.dma_keys[dma_key] = dict(sem=None, count=0)
            self.dma_last[dma_key] = (e, idx)
        self.ops[e].append(op)
        self.order.append((e, idx))
        return (e, idx)

    def emit(self):
        nc = self.nc
        esem = {e: nc.alloc_semaphore("s_" + e) for e in ENGS}
        for i, (k, d) in enumerate(self.dma_keys.items()):
            d["sem"] = nc.alloc_semaphore("d_%d" % i)
        for e in ENGS:
            c = 0
            for op in self.ops[e]:
                if op["dma_key"] is not None:
                    d = self.dma_keys[op["dma_key"]]
                    d["count"] += 1
                    op["val"] = (d["sem"], 16 * d["count"])
                elif op["signal"]:
                    c += 1
                    op["val"] = (esem[e], c)
        waited = {e: {} for e in ENGS}
        for (e, idx) in self.order:
            op = self.ops[e][idx]
            eng = self.eng[e]
            need = {}
            for (de, di) in op["deps"]:
                sem, val = self.ops[de][di]["val"]
                key = sem.num
                if key not in need or need[key][1] < val:
                    need[key] = (sem, val)
            for key, (sem, val) in need.items():
                if waited[e].get(key, 0) >= val:
                    continue
                waited[e][key] = val
                eng.wait_ge(sem, val)
            ins = op["fn"](eng)
            if op["val"] is not None:
                sem, val = op["val"]
                ins.then_inc(sem, 16 if op["dma_key"] is not None else 1)

    def final_wait(self, e, ops):
        eng = self.eng[e]
        for (de, di) in ops:
            sem, val = self.ops[de][di]["val"]
            eng.wait_ge(sem, val)


def run_threads(gens):
    gens = list(gens)
    while gens:
        nxt = []
        for g in gens:
            try:
                next(g)
                nxt.append(g)
            except StopIteration:
                pass
        gens = nxt


def build(stage=99, dbg=()):
    nc = bass.Bass("TRN2", target_bir_lowering=False)
    S = Sched(nc)
    din = {}

    def DI(name, shape):
        din[name] = nc.dram_tensor(name, list(shape), F32, kind="ExternalInput")
        return din[name]

    x_d = DI("x", [T, D])
    c_d = DI("c", [D])
    w_ada_d = DI("w_ada", [D, 6 * D])
    b_ada_d = DI("b_ada", [6 * D])
    g_mix_d = DI("g_mix", [D])
    w_in_d = DI("w_in", [D, IN_W])
    conv_w_d = DI("gdn_conv_w", [4, 3072])
    a_log_d = DI("gdn_a_log", [8])
    dt_bias_d = DI("gdn_dt_bias", [8])
    gdn_ng_d = DI("gdn_norm_g", [128])
    dil_qg_d = DI("dil_q_norm_g", [128])
    dil_kg_d = DI("dil_k_norm_g", [128])
    w_upg_d = DI("w_up_gdn", [D, D])
    w_upd_d = DI("w_up_dil", [512, D])
    w_out_d = DI("w_out", [D, D])
    g_ffn_d = DI("g_ffn", [D])
    w_r_d = DI("w_router", [D, 64])
    r_bias_d = DI("router_bias", [64])
    w_eg_d = DI("w_exp_gate", [64, D, 256])
    w_eu_d = DI("w_exp_up", [64, D, 256])
    w_ed_d = DI("w_exp_down", [64, 256, D])
    w_sg_d = DI("w_sh_gate", [D, 256])
    w_su_d = DI("w_sh_up", [D, 256])
    w_sd_d = DI("w_sh_down", [256, D])
    out_d = nc.dram_tensor("out", [T, D], F32, kind="ExternalOutput")
    dbg_out = {}
    finals = []

    def mm(out, lhsT, rhs, start=True, stop=True):
        S.add("pe", lambda e: e.matmul(out, lhsT=lhsT, rhs=rhs, start=start, stop=stop),
              reads=[lhsT, rhs], writes=[out])

    def tr(out, in_, idn):
        S.add("pe", lambda e: e.transpose(out=out, in_=in_, identity=idn),
              reads=[in_, idn], writes=[out])

    def act(out, in_, func, bias=None, scale=None, accum=None, eng="act"):
        rd = [in_]
        kw = {}
        if bias is not None:
            kw["bias"] = bias
            if not isinstance(bias, float):
                rd.append(bias)
        if scale is not None:
            kw["scale"] = scale
            if not isinstance(scale, float):
                rd.append(scale)
        wr = [out]
        if accum is not None:
            kw["accum_out"] = accum
            wr.append(accum)
        S.add(eng, lambda e: e.activation(out=out, in_=in_, func=func, **kw), reads=rd, writes=wr)

    def ts(out, in0, s1, s2=None, op0=ALU.mult, op1=None, eng="dve"):
        rd = [in0]
        if not isinstance(s1, float):
            rd.append(s1)
        if s2 is not None and not isinstance(s2, float):
            rd.append(s2)
        if op1 is None:
            S.add(eng, lambda e: e.tensor_scalar(out=out, in0=in0, scalar1=s1, scalar2=None, op0=op0),
                  reads=rd, writes=[out])
        else:
            S.add(eng, lambda e: e.tensor_scalar(out=out, in0=in0, scalar1=s1, scalar2=s2, op0=op0, op1=op1),
                  reads=rd, writes=[out])

    def tt(out, in0, in1, op, eng="dve"):
        S.add(eng, lambda e: e.tensor_tensor(out=out, in0=in0, in1=in1, op=op), reads=[in0, in1], writes=[out])

    def stt(out, in0, scalar, in1, op0, op1, eng="dve"):
        rd = [in0, in1]
        if not isinstance(scalar, float):
            rd.append(scalar)
        S.add(eng, lambda e: e.scalar_tensor_tensor(out=out, in0=in0, scalar=scalar, in1=in1, op0=op0, op1=op1),
              reads=rd, writes=[out])

    def cp(out, in_, eng="dve"):
        if eng == "act":
            act(out, in_, AF.Copy)
        else:
            S.add(eng, lambda e: e.tensor_copy(out=out, in_=in_), reads=[in_], writes=[out])

    def memset(ap, v, eng="pool"):
        S.add(eng, lambda e: e.memset(ap, v), writes=[ap])

    def recip(out, in_):
        S.add("dve", lambda e: e.reciprocal(out=out, in_=in_), reads=[in_], writes=[out])

    def dma(q, out, in_, key):
        return S.add(q, lambda e: e.dma_start(out=out, in_=in_), reads=[in_], writes=[out], dma_key=key)

    def dump(name, ap, shape):
        t = nc.dram_tensor("dbg_" + name, list(shape), ap.dtype, kind="ExternalOutput")
        dbg_out[name] = t
        finals.append(dma("sp", t.ap(), ap, "dbg_" + name))

    def finish():
        S.emit()
        S.final_wait("sp", finals)
        return nc, dbg_out

    class Arena:
        def __init__(self, base, limit):
            self.base, self.limit, self.cur = base, limit, base

        def a(self, name, shape, dtype):
            n = 1
            for s_ in shape[1:]:
                n *= s_
            nb = n * (4 if dtype == F32 else 2)
            nb = (nb + 31) // 32 * 32
            off = self.cur
            self.cur += nb
            assert self.cur <= self.limit, ("arena overflow", name, self.cur, self.limit)
            return S.sb(name, shape, dtype, off=off)

    KB = 1024
    B0 = 17 * KB
    CA = Arena(B0, B0 + 16 * KB)
    WA = Arena(B0 + 96 * KB, B0 + 207 * KB)

    banks = [S.ps("bank%d" % i, [128, 512], F32) for i in range(8)]
    bank_ctr = [0]

    held = set()

    def bank():
        for _ in range(8):
            i = bank_ctr[0] % 8
            bank_ctr[0] += 1
            if i not in held:
                return banks[i]
        raise RuntimeError("no free PSUM bank")

    def bacq():
        for _ in range(8):
            i = bank_ctr[0] % 8
            bank_ctr[0] += 1
            if i not in held:
                held.add(i)
                assert len(held) <= 8, "too many PSUM banks held"
                return banks[i]
        raise RuntimeError("no free PSUM bank")

    def brel(b):
        held.discard(banks.index(b))

    def bf(ap):
        return ap.bitcast(BF)

    ident = CA.a("ident", [128, 128], F32)
    identb = CA.a("identb", [128, 128], BF)
    ones_f = CA.a("ones_f", [128, 128], F32)
    ones_b = CA.a("ones_b", [128, 128], BF)
    memset(ident[:], 1.0)
    S.add("pool", lambda e: e.affine_select(out=ident[:], in_=ident[:], pattern=[[-1, 128]], compare_op=ALU.is_equal,
                                            fill=0.0, base=0, channel_multiplier=1), reads=[ident[:]], writes=[ident[:]])
    cp(identb[:], ident[:])
    memset(ones_f[:], 1.0)
    memset(ones_b[:], 1.0)
    blk2 = CA.a("blk2", [128, 128], F32)
    tri2 = CA.a("tri2", [128, 128], F32)
    mask2 = CA.a("mask2", [128, 2, 128], F32)
    half01 = CA.a("half01", [128, 2, 128], F32)
    memset(blk2[:], 0.0)
    memset(blk2[0:64, 0:64], 1.0)
    memset(blk2[64:128, 64:128], 1.0)
    S.add("pool", lambda e: e.affine_select(out=tri2[:], in_=blk2[:], pattern=[[1, 128]], compare_op=ALU.is_ge,
                                            fill=0.0, base=0, channel_multiplier=-1), reads=[blk2[:]], writes=[tri2[:]])
    cp(mask2[:, 0, :], tri2[:], eng="pool")
    S.add("pool", lambda e: e.affine_select(out=mask2[:, 1, :], in_=blk2[:], pattern=[[1, 128]], compare_op=ALU.is_ge,
                                            fill=0.0, base=-1, channel_multiplier=-1), reads=[blk2[:]], writes=[mask2[:, 1, :]])
    memset(half01[:], 0.0)
    memset(half01[0:64, 0, :], 1.0)
    memset(half01[64:128, 1, :], 1.0)
    amask = CA.a("amask", [128, 2, 128], BF)
    S.add("pool", lambda e: e.affine_select(out=amask[:, 0, :], in_=ones_b[:], pattern=[[-1, 128]], compare_op=ALU.is_ge,
                                            fill=0.0, base=0, channel_multiplier=1), reads=[ones_b[:]], writes=[amask[:, 0, :]])
    S.add("pool", lambda e: e.affine_select(out=amask[:, 1, :], in_=ones_b[:], pattern=[[1, 128]], compare_op=ALU.is_ge,
                                            fill=0.0, base=0, channel_multiplier=-1), reads=[ones_b[:]], writes=[amask[:, 1, :]])

    stg = WA.a("stg", [72, 128], F32)
    dma("sp", stg[0:8, :], c_d.ap().rearrange("(k p) -> k p", p=128), "st0")
    dma("sp", stg[8:16, :], g_mix_d.ap().rearrange("(k p) -> k p", p=128), "st1")
    dma("sp", stg[16:24, :], g_ffn_d.ap().rearrange("(k p) -> k p", p=128), "st2")
    dma("sp", stg[24:72, :], b_ada_d.ap().rearrange("(k p) -> k p", p=128), "st3")
    cols = CA.a("cols", [128, 72], F32)
    pb_ = bank()
    tr(pb_[:, 0:72], stg[:, :], ident[0:72, 0:72])
    cp(cols[:], pb_[:, 0:72])
    cs_b = CA.a("cs_b", [128, 8], BF)
    act(cs_b[:], cols[:, 0:8], AF.Silu)
    stg2 = WA.a("stg2", [96, 128], F32)
    dma("sp", stg2[:, :], conv_w_d.ap().rearrange("j (c p) -> (j c) p", p=128), "st4")
    convcol = CA.a("convcol", [128, 96], F32)
    pb_ = bank()
    tr(pb_[:, 0:96], stg2[:, :], ident[0:96, 0:96])
    cp(convcol[:], pb_[:, 0:96])
    rowc = CA.a("rowc", [128, 16 + 128 + 64], F32)
    dma("sp", rowc[:, 0:8], a_log_d.ap().partition_broadcast(128), "st5")
    dma("sp", rowc[:, 8:16], dt_bias_d.ap().partition_broadcast(128), "st6")
    dma("sp", rowc[:, 16:144], gdn_ng_d.ap().partition_broadcast(128), "st7")
    dma("sp", rowc[:, 144:208], r_bias_d.ap().partition_broadcast(128), "st8")
    dgcol = CA.a("dgcol", [128, 2], F32)
    dma("sp", dgcol[:, 0:1], dil_qg_d.ap().rearrange("(p o) -> p o", o=1), "st9")
    dma("sp", dgcol[:, 1:2], dil_kg_d.ap().rearrange("(p o) -> p o", o=1), "st10")
    ngcol = CA.a("ngcol", [128, 1], F32)
    dma("sp", ngcol[:, 0:1], gdn_ng_d.ap().rearrange("(p o) -> p o", o=1), "st11")

    hT = S.sb("hT", [128, 8, T], BF, off=B0 + 16 * KB)
    y_aT = S.sb("y_aT", [128, 8, T], BF, off=B0 + 48 * KB)
    y_bT = S.sb("y_bT", [128, 4, T], BF, off=B0 + 80 * KB)
    mod = CA.a("mod", [128, 48], F32)
    st1 = CA.a("st1", [128, 8], F32)
    wada = [WA.a("wada%d" % i, [128, 8, 512], BF) for i in range(2)]
    xt = [WA.a("xt%d" % i, [128, D], F32) for i in range(2)]
    xn = [WA.a("xn%d" % i, [128, D], F32) for i in range(2)]
    junk = WA.a("junk", [128, D], BF)
    xnT = WA.a("xnT", [128, 8, T], F32)
    pm = bacq()

    def x_tile(t):
        i = t % 2
        dma("sp", xt[i][:], x_d.ap()[t * 128:(t + 1) * 128, :], "xt%d" % i)
        ss = st1[:, 0:1]
        act(junk[:], xt[i][:], AF.Square, accum=ss)
        act(st1[:, 1:2], ss, AF.Sqrt, bias=EPS, scale=1.0 / D)
        recip(st1[:, 2:3], st1[:, 1:2])
        ts(xn[i][:], xt[i][:], st1[:, 2:3])
        for half in range(2):
            pt = bank()
            for q in range(4):
                kc = half * 4 + q
                tr(pt[:, q * 128:(q + 1) * 128], xn[i][:, kc * 128:(kc + 1) * 128], ident[:])
            cp(xnT[:, half * 4:(half + 1) * 4, t * 128:(t + 1) * 128],
               pt[:, :].rearrange("p (a b) -> p a b", b=128), eng=("act" if half == 0 else "dve"))

    tiles_at = [2, 1, 1, 2, 1, 1, 2, 1, 1, 2, 1, 1]
    tnext = 0
    for blk in range(12):
        wt = wada[blk % 2]
        dma("pool", wt[:], w_ada_d.ap()[:, blk * 512:(blk + 1) * 512].rearrange("(k p) n -> p k n", p=128), "wada%d" % (blk % 2))
        if blk >= 1:
            for _ in range(tiles_at[blk - 1]):
                x_tile(tnext)
                tnext += 1
        for j in range(4):
            col = blk * 4 + j
            for kc in range(8):
                mm(pm[:, col:col + 1], wt[:, kc, j * 128:(j + 1) * 128], cs_b[:, kc:kc + 1], start=(kc == 0), stop=(kc == 7))
    while tnext < NT:
        x_tile(tnext)
        tnext += 1
    tt(mod[:], pm[:, 0:48], cols[:, 24:72], ALU.add)
    brel(pm)
    AB = CA.a("AB", [128, 32], F32)
    stt(AB[:, 0:8], mod[:, 8:16], 1.0, cols[:, 8:16], ALU.add, ALU.mult)
    cp(AB[:, 8:16], mod[:, 0:8])
    stt(AB[:, 16:24], mod[:, 32:40], 1.0, cols[:, 16:24], ALU.add, ALU.mult)
    cp(AB[:, 24:32], mod[:, 24:32])
    for kc in range(8):
        if kc % 2 == 0:
            act(hT[:, kc, :], xnT[:, kc, :], AF.Identity, bias=AB[:, 8 + kc:9 + kc], scale=AB[:, kc:kc + 1])
        else:
            ts(hT[:, kc, :], xnT[:, kc, :], AB[:, kc:kc + 1], AB[:, 8 + kc:9 + kc], ALU.mult, ALU.add)
    gtB = CA.a("gtB", [128, 2, 1024], F32)
    dg = WA.a("dg", [128, 4, 128], F32)
    for gi, c0 in enumerate((16, 40)):
        for half in range(2):
            for q in range(4):
                kc = half * 4 + q
                ts(dg[:, q, :], ident[:], mod[:, c0 + kc:c0 + kc + 1])
            pg = bank()
            mm(pg[:, :], ones_f[:], dg[:].rearrange("p a b -> p (a b)"))
            cp(gtB[:, gi, half * 512:(half + 1) * 512], pg[:, :], eng="act")

    if "mod" in dbg:
        dump("mod", mod[:], [128, 48])
        dump("gtB", gtB[:], [128, 2, 1024])

    if "hT" in dbg:
        dump("hT", hT[:], [128, 8, T])
    if stage <= 1:
        return finish()

    S.barrier()
    WA.cur = WA.base
    wba = WA.a("wba", [128, 8, 16], BF)
    dma("pool", wba[:], w_in_d.ap()[:, OFF_GB:OFF_GB + 16].rearrange("(k p) n -> p k n", p=128), "wba")
    ba = WA.a("ba", [128, 16, 16], F32)
    pbk = bank()
    for t in range(NT):
        for kc in range(8):
            mm(pbk[:, t * 16:(t + 1) * 16], hT[:, kc, t * 128:(t + 1) * 128], wba[:, kc, :], start=(kc == 0), stop=(kc == 7))
    cp(ba[:], pbk[:, 0:256].rearrange("p (a b) -> p a b", b=16))
    NS = lambda nm: WA.a(nm, [128, 16, 8], F32)
    beta, lb, gg, gc, gcl, egc, edec, bg, egt0, egt1, tmp8 = [NS(n) for n in
        ("beta", "lb", "gg", "gc", "gcl", "egc", "edec", "bg", "egt0", "egt1", "tmp8")]
    nea = WA.a("nea", [128, 8], F32)
    act(beta[:], ba[:, :, 0:8], AF.Sigmoid)
    act(lb[:], beta[:], AF.Ln)
    tt(tmp8[:], ba[:, :, 8:16], rowc[:, None, 8:16].to_broadcast([128, 16, 8]), ALU.add)
    act(tmp8[:], tmp8[:], AF.Exp)
    act(tmp8[:], tmp8[:], AF.Ln, bias=1.0)
    act(nea[:], rowc[:, 0:8], AF.Exp)
    ts(nea[:], nea[:], -1.0)
    tt(gg[:], tmp8[:], nea[:, None, :].to_broadcast([128, 16, 8]), ALU.mult)
    pg = bank()
    g2 = gg[:].rearrange("p a b -> p (a b)")
    mm(pg[:, 0:128], tri2[:], g2)
    mm(pg[:, 128:256], blk2[:], g2)
    mm(pg[:, 256:384], half01[:, 0, :], g2)
    mm(pg[:, 384:512], half01[:, 1, :], g2)
    v3 = lambda ap: ap.rearrange("p (a b) -> p a b", b=8)
    cp(gc[:], v3(pg[:, 0:128]))
    tt(gcl[:], gc[:], lb[:], ALU.add)
    act(egc[:], gc[:], AF.Exp)
    tt(tmp8[:], v3(pg[:, 128:256]), gc[:], ALU.subtract)
    act(edec[:], tmp8[:], AF.Exp)
    tt(bg[:], beta[:], egc[:], ALU.mult)
    act(egt0[:], v3(pg[:, 256:384]), AF.Exp)
    act(egt1[:], v3(pg[:, 384:512]), AF.Exp)
    egt = (egt0, egt1)
    ngB = rowc[:, 16:144]

    if "gsc" in dbg:
        dump("beta", beta[:], [128, 16, 8])
        dump("gc", gc[:], [128, 16, 8])
        dump("egt0", egt0[:], [128, 16, 8])

    def so_set(i):
        d = {}
        d["qT"] = WA.a("so_qT%d" % i, [128, T], BF)
        d["wT"] = WA.a("so_wT%d" % i, [128, T], BF)
        d["u"] = WA.a("so_u%d" % i, [128, 16, 128], BF)
        d["qkT"] = WA.a("so_qkT%d" % i, [128, 16, 128], BF)
        d["kdec"] = WA.a("so_kdec%d" % i, [128, 16, 128], BF)
        d["zg"] = WA.a("so_zg%d" % i, [128, T], BF)
        d["nw"] = WA.a("so_nw%d" % i, [128, 16, 128], BF)
        return d

    SO = [so_set(0), so_set(1)]
    w4 = WA.a("w4", [128, 8, 4, 128], BF)
    xc = WA.a("xc", [128, 2048 + 16], BF)
    kT = WA.a("kT", [128, T], BF)
    vs = WA.a("vs", [128, T], BF)
    ktw = WA.a("ktw", [128, 16, 128], BF)
    vb = WA.a("vb", [128, 16, 128], BF)
    qs = WA.a("qs", [128, 512], F32)
    sq = WA.a("sq", [128, 512], BF)
    rt = WA.a("rt", [128, 512], F32)
    dgc = WA.a("dgc", [128, 4, 128], BF)
    NTT = 6
    for a_ in dbg:
        if a_.startswith("ntt"):
            NTT = int(a_[3:])
    TA = Arena(B0 + 80 * KB, B0 + 96 * KB)
    TMP = []
    for i in range(NTT):
        d = {}
        AR = TA if i >= 3 else WA
        d["X"] = AR.a("tX%d" % i, [128, 2, 128], F32)
        d["Dm"] = d["X"]
        d["E"] = d["X"]
        d["B"] = [AR.a("tB%d_%d" % (i, k), [128, 3, 128], F32) for k in range(2)]
        d["Gb"] = (WA if i == 5 else AR).a("tGb%d" % i, [128, 128], BF)
        TMP.append(d)
    Sf = TA.a("Sf", [128, 128], F32)
    negm = WA.a("negm", [128, 2, 128], F32)
    ts(negm[:], mask2[:], 1.0, 30000.0, ALU.subtract, ALU.mult)
    Sb2 = [TA.a("Sb2_%d" % i, [128, 128], BF) for i in range(2)]
    LA = 2
    RING = LA + 1
    MTr = [TA.a("MTr%d" % i, [128, 128], BF) for i in range(RING)]
    vn = TA.a("vn", [128, 128], BF)
    ot = [TA.a("ot%d" % i, [128, 128], F32) for i in range(2)]
    yt = TA.a("yt", [128, 128], BF)
    junkg = TA.a("junkg", [128, 128], BF)
    memset(xc[:, 0:3], 0.0)
    if "mem" in dbg:
        print("GDN WA used", (WA.cur - WA.base) / 1024, "of", (WA.limit - WA.base) / 1024, "TA used", (TA.cur - TA.base) / 1024)

    def gdn_tile(h, t, so, tm):
        X, Dm, E, B = tm["X"], tm["Dm"], tm["E"], tm["B"]
        tsl = slice(t * 128, (t + 1) * 128)
        ts(X[:, 0, :], ident[:], gc[:, t, h:h + 1])
        ts(X[:, 1, :], ident[:], gcl[:, t, h:h + 1])
        yield
        pR = bacq()
        mm(pR[:, 0:256], ones_f[:], X[:].rearrange("p a b -> p (a b)"))
        mm(pR[:, 256:384], kT[:, tsl], so["qT"][:, tsl])
        mm(pR[:, 384:512], kT[:, tsl], kT[:, tsl])
        yield
        stt(Dm[:].rearrange("p a b -> p (a b)"), pR[:, 0:256], gc[:, t, h:h + 1],
            negm[:].rearrange("p a b -> p (a b)"), ALU.subtract, ALU.add)
        yield
        act(E[:], Dm[:], AF.Exp)
        yield
        tt(so["qkT"][:, t, :], pR[:, 256:384], E[:, 0, :], ALU.mult)
        tt(B[0][:, 0, :], pR[:, 384:512], E[:, 1, :], ALU.mult)
        brel(pR)
        yield
        pT = bacq()
        tr(pT[:, 0:128], B[0][:, 0, :], ident[:])
        yield
        cp(B[0][:, 2, :], pT[:, 0:128], eng="act")
        tt(B[1][:, 1, :], ident[:], B[0][:, 0, :], ALU.subtract, eng="pool")
        brel(pT)
        yield
        pa = bacq()
        mm(pa[:, 0:128], B[0][:, 2, :], B[0][:, 0, :])
        mm(pa[:, 256:384], B[0][:, 0, :], B[0][:, 2, :])
        yield
        pav = pa[:, 0:384].rearrange("p (a b) -> p a b", b=128)
        cp(B[1][:, 0:3:2, :], pav[:, 0:3:2, :], eng="act")
        brel(pa)
        yield
        cur = 1
        for k in range(1, 5):
            nxt = 1 - cur
            pa = bacq()
            mm(pa[:, 0:256], B[cur][:, 2, :], B[cur][:, 0:2, :].rearrange("p a b -> p (a b)"))
            mm(pa[:, 256:384], B[cur][:, 0, :], B[cur][:, 2, :])
            yield
            pav = pa[:, 0:384].rearrange("p (a b) -> p a b", b=128)
            cp(B[nxt][:, 0:3:2, :], pav[:, 0:3:2, :], eng="act")
            tt(B[nxt][:, 1, :], B[cur][:, 1, :], pa[:, 128:256], ALU.add)
            brel(pa)
            cur = nxt
            yield
        pa = bacq()
        mm(pa[:, 0:128], B[cur][:, 2, :], B[cur][:, 1, :])
        yield
        G = tm["Gb"][:, :]
        tt(G, B[cur][:, 1, :], pa[:, 0:128], ALU.add)
        brel(pa)
        yield
        pu = bacq()
        mm(pu[:, 0:128], G, vb[:, t, :])
        mm(pu[:, 128:256], ktw[:, t, :], G)
        mm(pu[:, 256:384], G, ktw[:, t, :])
        yield
        cp(so["u"][:, t, :], pu[:, 0:128], eng="act")
        cp(so["wT"][:, tsl], pu[:, 128:256])
        ts(so["nw"][:, t, :], pu[:, 256:384], -1.0)
        brel(pu)
        yield

    def w4_load(h):
        for j, off in enumerate((OFF_GQ, OFF_GK, OFF_GV, OFF_GZ)):
            dma("pool", w4[:, :, j, :], w_in_d.ap()[:, off + h * 128: off + (h + 1) * 128].rearrange("(k p) n -> p k n", p=128), "w4_%d" % j)

    def gdn_pre(h, so):
        for jj in range(3):
            ct = jj * 8 + h
            for tap in range(4):
                ts(dgc[:, tap, :], ident[:], convcol[:, tap * 24 + ct: tap * 24 + ct + 1])
            for tb in range(4):
                pb = bank()
                for kc in range(8):
                    mm(pb[:, :], w4[:, kc, jj, :], hT[:, kc, tb * 512:(tb + 1) * 512], start=(kc == 0), stop=(kc == 7))
                cp(xc[:, 3 + tb * 512: 3 + (tb + 1) * 512], pb[:, :], eng="act")
                if tb % 2 == 1:
                    yield
            for tb in range(4):
                pb = bank()
                for tap in range(4):
                    mm(pb[:, :], dgc[:, tap, :], xc[:, tb * 512 + tap: tb * 512 + tap + 512], start=(tap == 0), stop=(tap == 3))
                bsl = slice(tb * 512, (tb + 1) * 512)
                if jj == 2:
                    act(vs[:, bsl], pb[:, :], AF.Silu)
                else:
                    act(qs[:], pb[:, :], AF.Silu)
                    act(sq[:], qs[:], AF.Square)
                    p2 = bank()
                    mm(p2[:, :], ones_b[:], sq[:])
                    act(rt[:], p2[:, :], AF.Sqrt, bias=EPS)
                    recip(rt[:], rt[:])
                    if jj == 0:
                        stt(so["qT"][:, bsl], qs[:], float(128 ** -0.5), rt[:], ALU.mult, ALU.mult)
                    else:
                        tt(kT[:, bsl], qs[:], rt[:], ALU.mult)
                if tb % 2 == 1:
                    yield
        for tb in range(4):
            pb = bank()
            for kc in range(8):
                mm(pb[:, :], w4[:, kc, 3, :], hT[:, kc, tb * 512:(tb + 1) * 512], start=(kc == 0), stop=(kc == 7))
            act(qs[:], pb[:, :], AF.Silu)
            ts(so["zg"][:, tb * 512:(tb + 1) * 512], qs[:], ngcol[:, 0:1])
            if tb % 2 == 1:
                yield
        if h + 1 < NH:
            w4_load(h + 1)
        yield
        for g4 in range(4):
            pb = bank()
            pv = bf(pb[:, :])
            for q in range(4):
                t = g4 * 4 + q
                tr(pv[:, q * 128:(q + 1) * 128], kT[:, t * 128:(t + 1) * 128], identb[:])
                tr(pv[:, 512 + q * 128: 512 + (q + 1) * 128], vs[:, t * 128:(t + 1) * 128], identb[:])
            kv_ = pv[:, 0:512].rearrange("p (a b) -> p a b", b=128)
            vv_ = pv[:, 512:1024].rearrange("p (a b) -> p a b", b=128)
            g4s = slice(g4 * 4, (g4 + 1) * 4)
            tt(ktw[:, g4s, :], kv_, bg[:, g4s, h:h + 1].to_broadcast([128, 4, 128]), ALU.mult)
            tt(so["kdec"][:, g4s, :], kv_, edec[:, g4s, h:h + 1].to_broadcast([128, 4, 128]), ALU.mult)
            tt(vb[:, g4s, :], vv_, beta[:, g4s, h:h + 1].to_broadcast([128, 4, 128]), ALU.mult)
            yield
        pend = list(range(NT if "notile" not in dbg else 0))
        active = []
        free_tmp = list(range(NTT))
        while pend or active:
            if pend and free_tmp:
                ti = free_tmp.pop(0)
                active.append((gdn_tile(h, pend.pop(0), so, TMP[ti]), ti))
            nxt = []
            for g, ti in active:
                try:
                    next(g)
                    nxt.append((g, ti))
                except StopIteration:
                    free_tmp.append(ti)
            active = nxt
            yield

    def gdn_scan(h, so):
        memset(Sf[:], 0.0)
        memset(Sb2[0][:], 0.0)
        NS_ = 32

        def rng(n):
            t, half = divmod(n, 2)
            lo, hi = half * 64, half * 64 + 64
            return t, half, lo, hi

        for n0 in range(LA):
            t, half, lo, hi = rng(n0)
            pM = bacq()
            mm(pM[:, 0:128], so["nw"][lo:hi, t, :], so["kdec"][lo:hi, t, :])
            yield
            cp(MTr[n0 % RING][:], pM[:, 0:128], eng="act")
            brel(pM)
            yield
        later = {}

        def at(r, fn):
            later.setdefault(r, []).append(fn)

        pY = [None]

        def getY():
            if pY[0] is None:
                pY[0] = bacq()
            return pY[0]

        def fin_a(t):
            o_ = ot[t % 2]
            act(junkg[:], o_[:], AF.Square, accum=st1[:, 3:4])
            act(st1[:, 4:5], st1[:, 3:4], AF.Ln, bias=EPS, scale=1.0 / 128)
            act(st1[:, 5:6], st1[:, 4:5], AF.Exp, scale=-0.5)

        def fin_b(t):
            o_ = ot[t % 2]
            ts(yt[:], o_[:], st1[:, 5:6])

        def fin_c(t):
            py = getY()
            tr(bf(py[:, :])[:, 512:640], yt[:], identb[:])

        def fin_d(t):
            py = getY()
            tt(y_aT[:, h, t * 128:(t + 1) * 128], bf(py[:, :])[:, 512:640], so["zg"][:, t * 128:(t + 1) * 128], ALU.mult)

        prev = None
        r = 0
        n = 0
        while n < NS_ or prev is not None or any(k >= r for k in later):
            pX = None
            cur = None
            if n < NS_:
                t, half, lo, hi = rng(n)
                tok = slice(t * 128 + lo, t * 128 + hi)
                Sc = Sb2[n % 2]
                pX = bacq()
                mm(pX[:, 256:384], so["kdec"][lo:hi, t, :], so["u"][lo:hi, t, :], start=True, stop=False)
                mm(pX[:, 256:384], MTr[n % RING][:], Sc[:, :], start=False, stop=True)
                mm(pX[lo:hi, 0:128], so["wT"][:, tok], Sc[:, :])
                mm(pX[lo:hi, 128:256], so["qT"][:, tok], Sc[:, :])
                cur = (n, t, half, lo, hi)
            if prev is not None:
                pn, pt_, phalf, plo, phi = prev
                if pX is None:
                    pX = bacq()
                mm(pX[plo:phi, 384:512], so["qkT"][plo:phi, pt_, plo:phi], vn[plo:phi, :])
            nla = n + LA
            if nla < NS_:
                t2, half2, lo2, hi2 = rng(nla)
                py = getY()
                mm(py[:, 0:128], so["nw"][lo2:hi2, t2, :], so["kdec"][lo2:hi2, t2, :])
            for fn in later.pop(r, []):
                fn()
            r += 1
            yield
            if cur is not None:
                Sn = Sb2[(n + 1) % 2]
                stt(Sn[:], Sf[:], egt[half][:, t, h:h + 1], pX[:, 256:384], ALU.mult, ALU.add)
                stt(Sf[:], Sf[:], egt[half][:, t, h:h + 1], pX[:, 256:384], ALU.mult, ALU.add)
            if prev is not None:
                pn, pt_, phalf, plo, phi = prev
                po_ = ot[pt_ % 2]
                tt(po_[plo:phi, :], po_[plo:phi, :], pX[plo:phi, 384:512], ALU.add)
                if phalf == 1:
                    fin_a(pt_)
                    at(r + 2, lambda pt_=pt_: fin_b(pt_))
                    at(r + 3, lambda pt_=pt_: fin_c(pt_))
                    at(r + 4, lambda pt_=pt_: fin_d(pt_))
            if cur is not None:
                o_ = ot[t % 2]
                tt(vn[lo:hi, :], so["u"][lo:hi, t, :], pX[lo:hi, 0:128], ALU.subtract)
                act(o_[lo:hi, :], pX[lo:hi, 128:256], AF.Copy, scale=egc[lo:hi, t, h:h + 1])
            if pX is not None:
                brel(pX)
            if nla < NS_:
                cp(MTr[nla % RING][:], pY[0][:, 0:128], eng="act")
            for fn in later.pop(r, []):
                fn()
            if pY[0] is not None:
                brel(pY[0])
                pY[0] = None
            r += 1
            prev = cur
            n += 1
            yield

    def multi(g, k):
        while True:
            for _ in range(k):
                try:
                    next(g)
                except StopIteration:
                    return
            yield

    NH = 8 if "gdn1" not in dbg else 1
    if "gdn2" in dbg:
        NH = 2
    w4_load(0)
    run_threads([gdn_pre(0, SO[0])])
    for h in range(NH):
        SM = 1
        for a_ in dbg:
            if a_.startswith("sm"):
                SM = int(a_[2:])
        th = [multi(gdn_scan(h, SO[h % 2]), SM)]
        if "noil" in dbg:
            run_threads(th)
            th = []
        if h + 1 < NH:
            th.append(gdn_pre(h + 1, SO[(h + 1) % 2]))
        run_threads(th)

    if "gdn" in dbg:
        dump("y_aT", y_aT[:, 0:NH, :], [128, NH, T])
        sd = SO[(NH - 1) % 2]
        dump("so_qT", sd["qT"][:], [128, T])
        dump("kT", kT[:], [128, T])
        dump("so_u", sd["u"][:], [128, 16, 128])
        dump("so_wT", sd["wT"][:], [128, T])
        dump("so_qkT", sd["qkT"][:], [128, 16, 128])
        dump("so_kdec", sd["kdec"][:], [128, 16, 128])
        dump("ktw", ktw[:], [128, 16, 128])
        dump("vb", vb[:], [128, 16, 128])
        dump("bg", bg[:], [128, 16, 8])
    if stage <= 2:
        return finish()

    S.barrier()
    WA.cur = WA.base
    ts(dgcol[:, 0:1], dgcol[:, 0:1], float(128 ** -0.5))
    DB = []
    for i in range(2):
        d = {}
        d["w"] = WA.a("dw%d" % i, [128, 8, 3, 128], BF)
        d["q"] = WA.a("dq%d" % i, [128, T], BF)
        d["k"] = WA.a("dk%d" % i, [128, T], BF)
        d["vt"] = WA.a("dvt%d" % i, [128, 16, 128], BF)
        DB.append(d)
    dvT = WA.a("dvT", [128, T], BF)
    sqd = WA.a("sqd", [128, 512], BF)
    rtd = WA.a("rtd", [128, 512], F32)
    acc = WA.a("acc", [128, 2, T], F32)
    Pf = [WA.a("Pf%d" % i, [128, 2, 128], BF) for i in range(6)]
    PTb = [WA.a("PTb%d" % i, [128, 2, 128], BF) for i in range(6)]
    DILS = (1, 4, 16)

    def dil_w_load(n):
        j, g = divmod(n, 3)
        hd = g * 4 + j
        for jj, off in enumerate((OFF_DQ, OFF_DK, OFF_DV)):
            dma("pool", DB[n % 2]["w"][:, :, jj, :], w_in_d.ap()[:, off + hd * 128: off + (hd + 1) * 128].rearrange("(k p) n -> p k n", p=128), "dw%d_%d" % (n % 2, jj))

    def dil_pre(n, db):
        j, g = divmod(n, 3)
        hd = g * 4 + j
        dil = DILS[g]
        if n + 1 < NDH:
            dil_w_load(n + 1)
        mb = 512 // dil
        for jj in range(3):
            dst = (db["q"], db["k"], dvT)[jj]
            for tb in range(4):
                pb = bank()
                for kc in range(8):
                    mm(pb[:, :], db["w"][:, kc, jj, :], hT[:, kc, tb * 512:(tb + 1) * 512], start=(kc == 0), stop=(kc == 7))
                if dil == 1:
                    dview = dst[:, tb * 512:(tb + 1) * 512]
                    sview = pb[:, :]
                    rview = rtd[:, :]
                else:
                    dview = dst[:, :].rearrange("p (r m) -> p r m", r=dil)[:, :, tb * mb:(tb + 1) * mb]
                    sview = pb[:, :].rearrange("p (m r) -> p r m", r=dil)
                    rview = rtd[:, :].rearrange("p (m r) -> p r m", r=dil)
                if jj == 2:
                    cp(dview, sview, eng="act")
                else:
                    act(sqd[:], pb[:, :], AF.Square)
                    p2 = bank()
                    mm(p2[:, :], ones_b[:], sqd[:])
                    act(rtd[:], p2[:, :], AF.Ln, bias=EPS, scale=1.0 / 128)
                    act(rtd[:], rtd[:], AF.Exp, scale=-0.5)
                    stt(dview, sview, dgcol[:, jj:jj + 1], rview, ALU.mult, ALU.mult)
                if True:
                    yield
        for g4 in range(4):
            pb = bank()
            pv = bf(pb[:, :])
            for q in range(4):
                t = g4 * 4 + q
                tr(pv[:, q * 128:(q + 1) * 128], dvT[:, t * 128:(t + 1) * 128], identb[:])
            cp(db["vt"][:, g4 * 4:(g4 + 1) * 4, :], pv[:, 0:512].rearrange("p (a b) -> p a b", b=128), eng="act")
            yield

    NDT = 6

    def dil_qtile(n, db, tq, ti):
        j, g = divmod(n, 3)
        dil = DILS[g]
        ntr = 16 // dil
        r, mt = divmod(tq, ntr)
        qs_ = slice(tq * 128, (tq + 1) * 128)
        P_, PT_ = Pf[ti], PTb[ti]
        pS = bacq()
        if mt > 0:
            mm(pS[:, 0:128], db["k"][:, (tq - 1) * 128: tq * 128], db["q"][:, qs_])
        mm(pS[:, 128:256], db["k"][:, qs_], db["q"][:, qs_])
        yield
        if mt > 0:
            act(P_[:].rearrange("p a b -> p (a b)"), pS[:, 0:256], AF.Exp)
        else:
            act(P_[:, 1, :], pS[:, 128:256], AF.Exp)
        brel(pS)
        yield
        if mt > 0:
            tt(PT_[:], P_[:], amask[:], ALU.mult, eng="pool")
        else:
            tt(PT_[:, 1, :], P_[:, 1, :], amask[:, 1, :], ALU.mult, eng="pool")
        yield
        pN = bacq()
        if mt > 0:
            mm(pN[:, 0:128], db["vt"][:, tq - 1, :], PT_[:, 0, :], start=True, stop=False)
        mm(pN[:, 0:128], db["vt"][:, tq, :], PT_[:, 1, :], start=(mt == 0), stop=True)
        if mt > 0:
            mm(pN[:, 128:256], ones_b[:], PT_[:, 0, :], start=True, stop=False)
        mm(pN[:, 128:256], ones_b[:], PT_[:, 1, :], start=(mt == 0), stop=True)
        yield
        st = r + mt * 128 * dil
        aview = acc[:, :, st: st + 127 * dil + 1: dil]
        nview = pN[:, 0:256].rearrange("p (a b) -> p a b", b=128)
        if g == 0:
            cp(aview, nview, eng="act")
        else:
            tt(aview, aview, nview, ALU.add)
        brel(pN)
        yield

    def dil_att(n, db):
        j, g = divmod(n, 3)
        pend = list(range(16))
        active = []
        free_t = list(range(NDT))
        while pend or active:
            if pend and free_t:
                ti = free_t.pop(0)
                active.append((dil_qtile(n, db, pend.pop(0), ti), ti))
            nxt = []
            for gq, ti in active:
                try:
                    next(gq)
                    nxt.append((gq, ti))
                except StopIteration:
                    free_t.append(ti)
            active = nxt
            yield
        if g == 2:
            recip(acc[:, 1, :], acc[:, 1, :])
            tt(y_bT[:, j, :], acc[:, 0, :], acc[:, 1, :], ALU.mult)
            yield

    NDH = 12 if "dil1" not in dbg else 3
    dil_w_load(0)
    run_threads([dil_pre(0, DB[0])])
    for n in range(NDH):
        th = [dil_att(n, DB[n % 2])]
        if n + 1 < NDH:
            th.append(dil_pre(n + 1, DB[(n + 1) % 2]))
        run_threads(th)
    if "dil" in dbg:
        dump("y_bT", y_bT[:, 0:NDH // 3, :], [128, NDH // 3, T])
    if stage <= 3:
        return finish()

    S.barrier()
    WA.cur = WA.base
    mergedT = WA.a("mergedT", [128, 8, T], BF)
    wg2 = [WA.a("wg2_%d" % i, [128, 8, 2, 128], BF) for i in range(2)]
    wupa = [WA.a("wupa%d" % i, [128, 8, 128], BF) for i in range(2)]
    wupd = [WA.a("wupd%d" % i, [128, 4, 128], BF) for i in range(2)]
    sga = [WA.a("sga%d" % i, [128, 512], F32) for i in range(2)]
    sgb = [WA.a("sgb%d" % i, [128, 512], F32) for i in range(2)]
    m1 = [WA.a("m1_%d" % i, [128, 512], BF) for i in range(2)]
    m2 = [WA.a("m2_%d" % i, [128, 512], BF) for i in range(2)]
    wo = WA.a("wo", [128, 8, D], BF)
    xt2 = [WA.a("xt2_%d" % i, [128, D], F32) for i in range(2)]
    for ft in range(8):
        i = ft % 2
        fsl = slice(ft * 128, (ft + 1) * 128)
        if ft == 2:
            dma("pool", wo[:], w_out_d.ap().rearrange("(k p) n -> p k n", p=128), "wo")
        if ft >= 4:
            for kc_ in (2 * (ft - 4), 2 * (ft - 4) + 1):
                tt(wo[:, kc_, :], wo[:, kc_, :], gtB[:, 0, :], ALU.mult, eng="pool")
        dma("pool", wg2[i][:, :, 0, :], w_in_d.ap()[:, OFF_GA_GATE + ft * 128: OFF_GA_GATE + (ft + 1) * 128].rearrange("(k p) n -> p k n", p=128), "wg2a%d" % i)
        dma("pool", wg2[i][:, :, 1, :], w_in_d.ap()[:, OFF_GB_GATE + ft * 128: OFF_GB_GATE + (ft + 1) * 128].rearrange("(k p) n -> p k n", p=128), "wg2b%d" % i)
        dma("pool", wupa[i][:], w_upg_d.ap()[:, fsl].rearrange("(k p) n -> p k n", p=128), "wupa%d" % i)
        dma("pool", wupd[i][:], w_upd_d.ap()[:, fsl].rearrange("(k p) n -> p k n", p=128), "wupd%d" % i)
        for tb in range(4):
            k2 = tb % 2
            bsl = slice(tb * 512, (tb + 1) * 512)
            pa, pb, pc, pd = bank(), bank(), bank(), bank()
            for kc in range(8):
                mm(pa[:, :], wg2[i][:, kc, 0, :], hT[:, kc, bsl], start=(kc == 0), stop=(kc == 7))
            for kc in range(8):
                mm(pb[:, :], wg2[i][:, kc, 1, :], hT[:, kc, bsl], start=(kc == 0), stop=(kc == 7))
            for hc in range(8):
                mm(pc[:, :], wupa[i][:, hc, :], y_aT[:, hc, bsl], start=(hc == 0), stop=(hc == 7))
            for sc_ in range(4):
                mm(pd[:, :], wupd[i][:, sc_, :], y_bT[:, sc_, bsl], start=(sc_ == 0), stop=(sc_ == 3))
            act(sga[k2][:], pa[:, :], AF.Sigmoid)
            act(sgb[k2][:], pb[:, :], AF.Sigmoid)
            tt(m1[k2][:], sga[k2][:], pc[:, :], ALU.mult)
            tt(m2[k2][:], sgb[k2][:], pd[:, :], ALU.mult)
            tt(mergedT[:, ft, bsl], m1[k2][:], m2[k2][:], ALU.add, eng="pool")
    if "mrg" in dbg:
        dump("mergedT", mergedT[:], [128, 8, T])
    S.barrier()
    x1 = S.sb("x1", [128, 16, D], F32, off=B0 + 16 * KB)
    for t in range(NT):
        i = t % 2
        dma("sp", xt2[i][:], x_d.ap()[t * 128:(t + 1) * 128, :], "xt2_%d" % i)
        for half in range(2):
            hs = slice(half * 512, (half + 1) * 512)
            pb = bank()
            for ft in range(8):
                mm(pb[:, :], mergedT[:, ft, t * 128:(t + 1) * 128], wo[:, ft, hs], start=(ft == 0), stop=(ft == 7))
            tt(x1[:, t, hs], xt2[i][:, hs], pb[:, :], ALU.add)
    if "x1" in dbg:
        dump("x1", x1[:], [128, 16, D])
    if stage <= 4:
        return finish()

    S.barrier()
    MA = Arena(B0 + 80 * KB, B0 + 207 * KB)
    h2T = MA.a("h2T", [128, 8, T], BF)
    cT = MA.a("cT", [64, T], BF)
    wgu = [MA.a("wgu%d" % i, [128, 8, 512], BF) for i in range(3)]
    wdn = [MA.a("wdn%d" % i, [128, 2, D], BF) for i in range(6)]
    sact = [MA.a("sact%d" % i, [128, 512], BF) for i in range(2)]
    tmid = [MA.a("tmid%d" % i, [128, 512], BF) for i in range(2)]
    sel_e = [MA.a("sel_e%d" % i, [64, 128], BF) for i in range(2)]
    mark = MA.cur
    hTe = [[MA.a("hTe%d_%d" % (a, b), [128, 2, T], BF) for b in range(2)] for a in range(2)]
    MA.cur = mark
    wr = MA.a("wr", [128, 8, 64], F32)
    dma("sp", wr[:], w_r_d.ap().rearrange("(k p) n -> p k n", p=128), "wr")
    rbias = rowc[:, 144:208]
    NRT = 3
    RT = []
    for i in range(NRT):
        d = {}
        d["xn"] = MA.a("xn2_%d" % i, [128, D], F32)
        d["junk"] = MA.a("junk2_%d" % i, [128, D], BF)
        d["h2f"] = MA.a("h2f_%d" % i, [128, 8, 128], F32)
        for nm in ("scr", "sel", "selm", "wkk", "comb", "emk"):
            d[nm] = MA.a("%s_%d" % (nm, i), [128, 64], F32)
        d["top"] = MA.a("top_%d" % i, [128, 8, 8], F32)
        for nm in ("gs", "gtop", "gm", "t30", "top8", "st"):
            d[nm] = MA.a("%s_%d" % (nm, i), [128, 8], F32)
        RT.append(d)

    def router_tile(t, d):
        src_ap = x1[:, t, :]
        st_ = d["st"]
        xn_, h2f = d["xn"], d["h2f"]
        act(d["junk"][:], src_ap, AF.Square, accum=st_[:, 0:1])
        yield
        act(st_[:, 1:2], st_[:, 0:1], AF.Sqrt, bias=EPS, scale=1.0 / D)
        yield
        recip(st_[:, 2:3], st_[:, 1:2])
        yield
        ts(xn_[:], src_ap, st_[:, 2:3])
        yield
        for half in range(2):
            pt = bacq()
            for q in range(4):
                kc = half * 4 + q
                tr(pt[:, q * 128:(q + 1) * 128], xn_[:, kc * 128:(kc + 1) * 128], ident[:])
            yield
            for q in range(4):
                kc = half * 4 + q
                if q % 2 == 0:
                    act(h2f[:, kc, :], pt[:, q * 128:(q + 1) * 128], AF.Identity,
                        bias=AB[:, 24 + kc:25 + kc], scale=AB[:, 16 + kc:17 + kc])
                else:
                    ts(h2f[:, kc, :], pt[:, q * 128:(q + 1) * 128], AB[:, 16 + kc:17 + kc],
                       AB[:, 24 + kc:25 + kc], ALU.mult, ALU.add)
            brel(pt)
            yield
            cp(h2T[:, half * 4:(half + 1) * 4, t * 128:(t + 1) * 128], h2f[:, half * 4:(half + 1) * 4, :], eng="pool")
        pl = bacq()
        for kc in range(8):
            mm(pl[:, 0:64], h2f[:, kc, :], wr[:, kc, :], start=(kc == 0), stop=(kc == 7))
        yield
        scr, sel, selm, wkk, comb, emk = [d[n] for n in ("scr", "sel", "selm", "wkk", "comb", "emk")]
        top, gs, gtop, gm, t30, top8 = [d[n] for n in ("top", "gs", "gtop", "gm", "t30", "top8")]
        act(scr[:], pl[:, 0:64], AF.Sigmoid)
        brel(pl)
        yield
        tt(sel[:], scr[:], rbias, ALU.add)
        yield
        for g in range(8):
            S.add("dve", lambda e, g=g: e.max(out=top[:, g, :], in_=sel[:, g * 8:(g + 1) * 8]),
                  reads=[sel[:, g * 8:(g + 1) * 8]], writes=[top[:, g, :]])
        yield
        tt(gs[:], top[:, :, 0], top[:, :, 1], ALU.add)
        yield
        S.add("dve", lambda e: e.max(out=gtop[:], in_=gs[:]), reads=[gs[:]], writes=[gtop[:]])
        yield
        ts(gm[:], gs[:], gtop[:, 3:4], None, ALU.is_ge)
        yield
        ts(t30[:], gm[:], 30.0, -30.0, ALU.mult, ALU.add)
        sel3 = sel[:].rearrange("p (a b) -> p a b", b=8)
        selm3 = selm[:].rearrange("p (a b) -> p a b", b=8)
        tt(selm3, sel3, gm[:, :, None].to_broadcast([128, 8, 8]), ALU.mult)
        yield
        tt(selm3, selm3, t30[:, :, None].to_broadcast([128, 8, 8]), ALU.add)
        yield
        S.add("dve", lambda e: e.max(out=top8[:], in_=selm[:]), reads=[selm[:]], writes=[top8[:]])
        yield
        ts(emk[:], selm[:], top8[:, 7:8], None, ALU.is_ge)
        yield
        tt(wkk[:], scr[:], emk[:], ALU.mult)
        yield
        S.add("dve", lambda e: e.reduce_sum(out=st_[:, 3:4], in_=wkk[:], axis=AX.X), reads=[wkk[:]], writes=[st_[:, 3:4]])
        yield
        recip(st_[:, 4:5], st_[:, 3:4])
        yield
        ts(comb[:], wkk[:], st_[:, 4:5], 2.5, ALU.mult, ALU.mult)
        yield
        pc = bacq()
        tr(pc[0:64, 0:128], comb[:], ident[:])
        yield
        cp(cT[:, t * 128:(t + 1) * 128], pc[0:64, 0:128], eng="act")
        brel(pc)
        yield

    groups = [(2 * g, 2 * g + 1) for g in range(32)] + [(64,)]
    if "moe1" in dbg:
        groups = groups[:2] + [(64,)]
    elist = [e for grp in groups for e in grp]

    def load_w(e, slot):
        wb = wgu[slot % 3]
        wd_ = wdn[slot % 6]
        if e < 64:
            dma("pool", wb[:, :, 0:256], w_eg_d.ap()[e].rearrange("(k p) n -> p k n", p=128), "wgu_g%d" % (slot % 3))
            dma("pool", wb[:, :, 256:512], w_eu_d.ap()[e].rearrange("(k p) n -> p k n", p=128), "wgu_u%d" % (slot % 3))
            dma("pool", wd_[:], w_ed_d.ap()[e].rearrange("(k p) n -> p k n", p=128), "wdn%d" % (slot % 6))
        else:
            dma("pool", wb[:, :, 0:256], w_sg_d.ap().rearrange("(k p) n -> p k n", p=128), "wgu_g%d" % (slot % 3))
            dma("pool", wb[:, :, 256:512], w_su_d.ap().rearrange("(k p) n -> p k n", p=128), "wgu_u%d" % (slot % 3))
            dma("pool", wd_[:], w_sd_d.ap().rearrange("(k p) n -> p k n", p=128), "wdn%d" % (slot % 6))

    load_w(elist[0], 0)
    load_w(elist[1], 1)

    pend_t = list(range(NT))
    active = []
    free_r = list(range(NRT))
    while pend_t or active:
        if pend_t and free_r:
            ri = free_r.pop(0)
            active.append((router_tile(pend_t.pop(0), RT[ri]), ri))
        nxt_ = []
        for g_, ri in active:
            try:
                next(g_)
                nxt_.append((g_, ri))
            except StopIteration:
                free_r.append(ri)
        active = nxt_
    if "rt" in dbg:
        dump("h2T", h2T[:], [128, 8, T])
        dump("cT", cT[:], [64, T])
    if stage <= 5:
        return finish()

    S.barrier()


    def prep_w(e, slot):
        wd_ = wdn[slot % 6]
        tt(wd_[:], wd_[:], gtB[:, 1:2, :].to_broadcast([128, 2, D]), ALU.mult)
        if e < 64:
            ts(sel_e[slot % 2][:], ones_b[0:64, :], ident[0:64, e:e + 1])

    def gateup(e, slot, gi):
        wb = wgu[slot % 3]
        wd_ = wdn[slot % 6]
        if slot + 2 < len(elist):
            load_w(elist[slot + 2], slot + 2)
        he = hTe[gi % 2][slot % 2]
        se = sel_e[slot % 2]
        for tb in range(4):
            if tb == 1 and slot + 1 < len(elist):
                prep_w(elist[slot + 1], slot + 1)
            bsl = slice(tb * 512, (tb + 1) * 512)
            if e < 64:
                pcb = bank()
                mm(pcb[:, :], se[:], cT[:, bsl])
            for ft in range(2):
                k2 = (tb * 2 + ft) % 2
                pa, pu = bank(), bank()
                for kc in range(8):
                    mm(pa[:, :], wb[:, kc, ft * 128:(ft + 1) * 128], h2T[:, kc, bsl], start=(kc == 0), stop=(kc == 7))
                for kc in range(8):
                    mm(pu[:, :], wb[:, kc, 256 + ft * 128: 256 + (ft + 1) * 128], h2T[:, kc, bsl], start=(kc == 0), stop=(kc == 7))
                act(sact[k2][:], pa[:, :], AF.Silu)
                if e < 64:
                    tt(tmid[k2][:], sact[k2][:], pu[:, :], ALU.mult)
                    tt(he[:, ft, bsl], tmid[k2][:], pcb[:, :], ALU.mult)
                else:
                    tt(he[:, ft, bsl], sact[k2][:], pu[:, :], ALU.mult)
        return he, wd_

    def down(items, last):
        for t in range(NT):
            for half in range(2):
                hs = slice(half * 512, (half + 1) * 512)
                pb = bank()
                n = len(items) * 2
                k = 0
                for (he, wd_) in items:
                    for ft in range(2):
                        mm(pb[:, :], he[:, ft, t * 128:(t + 1) * 128], wd_[:, ft, hs], start=(k == 0), stop=(k == n - 1))
                        k += 1
                tt(x1[:, t, hs], x1[:, t, hs], pb[:, :], ALU.add)
            if last:
                finals.append(dma("sp", out_d.ap()[t * 128:(t + 1) * 128, :], x1[:, t, :], "out%d" % (t % 4)))

    prep_w(elist[0], 0)
    slot = 0
    prev = None
    for gi, grp in enumerate(groups):
        items = []
        for e in grp:
            items.append(gateup(e, slot, gi))
            slot += 1
        if prev is not None:
            down(prev, False)
        prev = items
    down(prev, True)
    return finish()


_IN_NAMES = ["x", "c", "w_ada", "b_ada", "g_mix", "w_in", "gdn_conv_w", "gdn_a_log", "gdn_dt_bias", "gdn_norm_g",
             "dil_q_norm_g", "dil_k_norm_g", "w_up_gdn", "w_up_dil", "w_out", "g_ffn", "w_router", "router_bias",
             "w_exp_gate", "w_exp_up", "w_exp_down", "w_sh_gate", "w_sh_up", "w_sh_down"]


def make_in_maps(inputs, n_cores=8):
    shared = {}
    for k in _IN_NAMES:
        if k in ("x", "c"):
            continue
        a = np.asarray(inputs[k], dtype=np.float32)
        shared[k] = np.ascontiguousarray(a[0])
    maps = []
    for b in range(n_cores):
        m = dict(shared)
        m["x"] = np.ascontiguousarray(np.asarray(inputs["x"], dtype=np.float32)[b])
        m["c"] = np.ascontiguousarray(np.asarray(inputs["c"], dtype=np.float32)[b])
        maps.append(m)
    return maps


def kernel(**inputs):
    nc, _ = build()
    maps = make_in_maps(inputs)
    res = run_bass_kernel_spmd(nc, maps, core_ids=list(range(8)))
    out = np.stack([np.asarray(r["out"], dtype=np.float32) for r in res.results], axis=0)
    return out
```

```python
import numpy as np
import concourse.bass as bass
import concourse.mybir as mybir
from concourse.bass_utils import run_bass_kernel_spmd

F32 = mybir.dt.float32
BF = mybir.dt.bfloat16
AF = mybir.ActivationFunctionType
ALU = mybir.AluOpType
AX = mybir.AxisListType

ENGS = ("pe", "act", "dve", "pool", "sp")

T = 2048
D = 1024
NT = 16
KC = 8
IN_W = 10768
EPS = 1e-6
N_EXP = 64
OFF_GQ, OFF_GK, OFF_GV, OFF_GZ, OFF_GB, OFF_GA = 0, 1024, 2048, 3072, 4096, 4104
OFF_DQ, OFF_DK, OFF_DV, OFF_GA_GATE, OFF_GB_GATE = 4112, 5648, 7184, 8720, 9744


class Sched:
    def __init__(self, nc):
        self.nc = nc
        self.eng = {"pe": nc.tensor, "act": nc.scalar, "dve": nc.vector,
                    "pool": nc.gpsimd, "sp": nc.sync}
        self.ops = {e: [] for e in ENGS}
        self.order = []
        self.fsz = {}
        self.kind = {}
        self.recs = {}
        self.dma_keys = {}
        self.dma_last = {}
        self.pending = {}

    def barrier(self):
        last = set()
        for e in ENGS:
            if self.ops[e]:
                last.add((e, len(self.ops[e]) - 1))
        for k, v in self.dma_last.items():
            last.add(v)
        for (de, di) in last:
            self.ops[de][di]["signal"] = True
        for e in ENGS:
            self.pending[e] = set(last) | self.pending.get(e, set())

    def sb(self, name, shape, dtype, off=None):
        if off is None:
            raise RuntimeError("explicit offset required")
        t = self.nc.alloc_sbuf_tensor_at(name, list(shape), dtype, offset=off)
        f = 1
        for s in shape[1:]:
            f *= s
        self.fsz[t.name] = f
        self.kind[t.name] = "sb"
        self.recs[t.name] = []
        return t

    def ps(self, name, shape, dtype):
        t = self.nc.alloc_psum_tensor(name, list(shape), dtype)
        f = 1
        for s in shape[1:]:
            f *= s
        self.fsz[t.name] = f
        self.kind[t.name] = "ps"
        self.recs[t.name] = []
        return t

    def box(self, ap):
        name = ap.tensor.name
        if name not in self.fsz:
            return None
        f = self.fsz[name]
        if self.kind[name] == "ps":
            return (name, 0, 128, 0, f)
        off = ap.offset
        dims = ap.ap
        plo = off // f
        phi = plo + dims[0][1]
        flo = off % f
        ext = 1
        for st, cnt in dims[1:]:
            ext += (cnt - 1) * abs(st)
        return (name, plo, phi, flo, flo + ext)

    def add(self, e, fn, reads=(), writes=(), dma_key=None):
        idx = len(self.ops[e])
        op = dict(fn=fn, deps=set(), dma_key=dma_key, signal=False, val=None)
        rb = [self.box(a) for a in reads]
        wb = [self.box(a) for a in writes]
        deps = set()
        for b in rb:
            if b is None:
                continue
            name, plo, phi, flo, fhi = b
            isps = self.kind[name] == "ps"
            for r in self.recs[name]:
                if (r[6] or isps) and r[0] < phi and plo < r[1] and r[2] < fhi and flo < r[3]:
                    deps.add((r[4], r[5]))
        for b in wb:
            if b is None:
                continue
            name, plo, phi, flo, fhi = b
            for r in self.recs[name]:
                if r[0] < phi and plo < r[1] and r[2] < fhi and flo < r[3]:
                    deps.add((r[4], r[5]))
        for b in wb:
            if b is None:
                continue
            name, plo, phi, flo, fhi = b
            lst = self.recs[name]
            lst[:] = [r for r in lst if not (plo <= r[0] and r[1] <= phi and flo <= r[2] and r[3] <= fhi)]
            lst.append([plo, phi, flo, fhi, e, idx, True])
        for b in rb:
            if b is None:
                continue
            name, plo, phi, flo, fhi = b
            lst = self.recs[name]
            if self.kind[name] == "ps":
                lst[:] = [[plo, phi, flo, fhi, e, idx, True]]
            else:
                lst[:] = [r for r in lst if not ((not r[6]) and r[4] == e and dma_key is None and r[5] < idx
                                                 and self.ops[e][r[5]]["dma_key"] is None
                                                 and plo <= r[0] and r[1] <= phi and flo <= r[2] and r[3] <= fhi)]
                lst.append([plo, phi, flo, fhi, e, idx, False])
        if e in self.pending:
            deps |= self.pending.pop(e)
        if dma_key is not None and dma_key in self.dma_last:
            deps.add(self.dma_last[dma_key])
        fdeps = set()
        best = {}
        for (de, di) in deps:
            dop = self.ops[de][di]
            if de == e and e == "pe" and dop["dma_key"] is None and dma_key is None:
                continue
            if dop["dma_key"] is not None:
                fdeps.add((de, di))
            elif best.get(de, -1) < di:
                best[de] = di
        for de, di in best.items():
            fdeps.add((de, di))
        for (de, di) in fdeps:
            self.ops[de][di]["signal"] = True
        op["deps"] = fdeps
        if dma_key is not None:
            op["signal"] = True
            if dma_key not in self.dma_keys:
                self.dma_keys[dma_key] = dict(sem=None, count=0)
            self.dma_last[dma_key] = (e, idx)
        self.ops[e].append(op)
        self.order.append((e, idx))
        return (e, idx)

    def emit(self):
        nc = self.nc
        esem = {e: nc.alloc_semaphore("s_" + e) for e in ENGS}
        for i, (k, d) in enumerate(self.dma_keys.items()):
            d["sem"] = nc.alloc_semaphore("d_%d" % i)
        for e in ENGS:
            c = 0
            for op in self.ops[e]:
                if op["dma_key"] is not None:
                    d = self.dma_keys[op["dma_key"]]
                    d["count"] += 1
                    op["val"] = (d["sem"], 16 * d["count"])
                elif op["signal"]:
                    c += 1
                    op["val"] = (esem[e], c)
        waited = {e: {} for e in ENGS}
        for (e, idx) in self.order:
            op = self.ops[e][idx]
            eng = self.eng[e]
            need = {}
            for (de, di) in op["deps"]:
                sem, val = self.ops[de][di]["val"]
                key = sem.num
                if key not in need or need[key][1] < val:
                    need[key] = (sem, val)
            for key, (sem, val) in need.items():
                if waited[e].get(key, 0) >= val:
                    continue
                waited[e][key] = val
                eng.wait_ge(sem, val)
            ins = op["fn"](eng)
            if op["val"] is not None:
                sem, val = op["val"]
                ins.then_inc(sem, 16 if op["dma_key"] is not None else 1)

    def final_wait(self, e, ops):
        eng = self.eng[e]
        for (de, di) in ops:
            sem, val = self.ops[de][di]["val"]
            eng.wait_ge(sem, val)


def run_threads(gens):
    gens = list(gens)
    while gens:
        nxt = []
        for g in gens:
            try:
                next(g)
                nxt.append(g)
            except StopIteration:
                pass
        gens = nxt


def build(stage=99, dbg=()):
    nc = bass.Bass("TRN2", target_bir_lowering=False)
    S = Sched(nc)
    din = {}

    def DI(name, shape):
        din[name] = nc.dram_tensor(name, list(shape), F32, kind="ExternalInput")
        return din[name]

    x_d = DI("x", [T, D])
    c_d = DI("c", [D])
    w_ada_d = DI("w_ada", [D, 6 * D])
    b_ada_d = DI("b_ada", [6 * D])
    g_mix_d = DI("g_mix", [D])
    w_in_d = DI("w_in", [D, IN_W])
    conv_w_d = DI("gdn_conv_w", [4, 3072])
    a_log_d = DI("gdn_a_log", [8])
    dt_bias_d = DI("gdn_dt_bias", [8])
    gdn_ng_d = DI("gdn_norm_g", [128])
    dil_qg_d = DI("dil_q_norm_g", [128])
    dil_kg_d = DI("dil_k_norm_g", [128])
    w_upg_d = DI("w_up_gdn", [D, D])
    w_upd_d = DI("w_up_dil", [512, D])
    w_out_d = DI("w_out", [D, D])
    g_ffn_d = DI("g_ffn", [D])
    w_r_d = DI("w_router", [D, 64])
    r_bias_d = DI("router_bias", [64])
    w_eg_d = DI("w_exp_gate", [64, D, 256])
    w_eu_d = DI("w_exp_up", [64, D, 256])
    w_ed_d = DI("w_exp_down", [64, 256, D])
    w_sg_d = DI("w_sh_gate", [D, 256])
    w_su_d = DI("w_sh_up", [D, 256])
    w_sd_d = DI("w_sh_down", [256, D])
    out_d = nc.dram_tensor("out", [T, D], F32, kind="ExternalOutput")
    dbg_out = {}
    finals = []

    def mm(out, lhsT, rhs, start=True, stop=True):
        S.add("pe", lambda e: e.matmul(out, lhsT=lhsT, rhs=rhs, start=start, stop=stop),
              reads=[lhsT, rhs], writes=[out])

    def tr(out, in_, idn):
        S.add("pe", lambda e: e.transpose(out=out, in_=in_, identity=idn),
              reads=[in_, idn], writes=[out])

    def act(out, in_, func, bias=None, scale=None, accum=None, eng="act"):
        rd = [in_]
        kw = {}
        if bias is not None:
            kw["bias"] = bias
            if not isinstance(bias, float):
                rd.append(bias)
        if scale is not None:
            kw["scale"] = scale
            if not isinstance(scale, float):
                rd.append(scale)
        wr = [out]
        if accum is not None:
            kw["accum_out"] = accum
            wr.append(accum)
        S.add(eng, lambda e: e.activation(out=out, in_=in_, func=func, **kw), reads=rd, writes=wr)

    def ts(out, in0, s1, s2=None, op0=ALU.mult, op1=None, eng="dve"):
        rd = [in0]
        if not isinstance(s1, float):
            rd.append(s1)
        if s2 is not None and not isinstance(s2, float):
            rd.append(s2)
        if op1 is None:
            S.add(eng, lambda e: e.tensor_scalar(out=out, in0=in0, scalar1=s1, scalar2=None, op0=op0),
                  reads=rd, writes=[out])
        else:
            S.add(eng, lambda e: e.tensor_scalar(out=out, in0=in0, scalar1=s1, scalar2=s2, op0=op0, op1=op1),
                  reads=rd, writes=[out])

    def tt(out, in0, in1, op, eng="dve"):
        S.add(eng, lambda e: e.tensor_tensor(out=out, in0=in0, in1=in1, op=op), reads=[in0, in1], writes=[out])

    def stt(out, in0, scalar, in1, op0, op1, eng="dve"):
        rd = [in0, in1]
        if not isinstance(scalar, float):
            rd.append(scalar)
        S.add(eng, lambda e: e.scalar_tensor_tensor(out=out, in0=in0, scalar=scalar, in1=in1, op0=op0, op1=op1),
              reads=rd, writes=[out])

    def cp(out, in_, eng="dve"):
        if eng == "act":
            act(out, in_, AF.Copy)
        else:
            S.add(eng, lambda e: e.tensor_copy(out=out, in_=in_), reads=[in_], writes=[out])

    def memset(ap, v, eng="pool"):
        S.add(eng, lambda e: e.memset(ap, v), writes=[ap])

    def recip(out, in_):
        S.add("dve", lambda e: e.reciprocal(out=out, in_=in_), reads=[in_], writes=[out])

    def dma(q, out, in_, key):
        return S.add(q, lambda e: e.dma_start(out=out, in_=in_), reads=[in_], writes=[out], dma_key=key)

    def dump(name, ap, shape):
        t = nc.dram_tensor("dbg_" + name, list(shape), ap.dtype, kind="ExternalOutput")
        dbg_out[name] = t
        finals.append(dma("sp", t.ap(), ap, "dbg_" + name))

    def finish():
        S.emit()
        S.final_wait("sp", finals)
        return nc, dbg_out

    class Arena:
        def __init__(self, base, limit):
            self.base, self.limit, self.cur = base, limit, base

        def a(self, name, shape, dtype):
            n = 1
            for s_ in shape[1:]:
                n *= s_
            nb = n * (4 if dtype == F32 else 2)
            nb = (nb + 31) // 32 * 32
            off = self.cur
            self.cur += nb
            assert self.cur <= self.limit, ("arena overflow", name, self.cur, self.limit)
            return S.sb(name, shape, dtype, off=off)

    KB = 1024
    B0 = 17 * KB
    CA = Arena(B0, B0 + 16 * KB)
    WA = Arena(B0 + 96 * KB, B0 + 207 * KB)

    banks = [S.ps("bank%d" % i, [128, 512], F32) for i in range(8)]
    bank_ctr = [0]

    held = set()

    def bank():
        for _ in range(8):
            i = bank_ctr[0] % 8
            bank_ctr[0] += 1
            if i not in held:
                return banks[i]
        raise RuntimeError("no free PSUM bank")

    def bacq():
        for _ in range(8):
            i = bank_ctr[0] % 8
            bank_ctr[0] += 1
            if i not in held:
                held.add(i)
                assert len(held) <= 8, "too many PSUM banks held"
                return banks[i]
        raise RuntimeError("no free PSUM bank")

    def brel(b):
        held.discard(banks.index(b))

    def bf(ap):
        return ap.bitcast(BF)

    ident = CA.a("ident", [128, 128], F32)
    identb = CA.a("identb", [128, 128], BF)
    ones_f = CA.a("ones_f", [128, 128], F32)
    ones_b = CA.a("ones_b", [128, 128], BF)
    memset(ident[:], 1.0)
    S.add("pool", lambda e: e.affine_select(out=ident[:], in_=ident[:], pattern=[[-1, 128]], compare_op=ALU.is_equal,
                                            fill=0.0, base=0, channel_multiplier=1), reads=[ident[:]], writes=[ident[:]])
    cp(identb[:], ident[:])
    memset(ones_f[:], 1.0)
    memset(ones_b[:], 1.0)
    blk2 = CA.a("blk2", [128, 128], F32)
    tri2 = CA.a("tri2", [128, 128], F32)
    mask2 = CA.a("mask2", [128, 2, 128], F32)
    half01 = CA.a("half01", [128, 2, 128], F32)
    memset(blk2[:], 0.0)
    memset(blk2[0:64, 0:64], 1.0)
    memset(blk2[64:128, 64:128], 1.0)
    S.add("pool", lambda e: e.affine_select(out=tri2[:], in_=blk2[:], pattern=[[1, 128]], compare_op=ALU.is_ge,
                                            fill=0.0, base=0, channel_multiplier=-1), reads=[blk2[:]], writes=[tri2[:]])
    cp(mask2[:, 0, :], tri2[:], eng="pool")
    S.add("pool", lambda e: e.affine_select(out=mask2[:, 1, :], in_=blk2[:], pattern=[[1, 128]], compare_op=ALU.is_ge,
                                            fill=0.0, base=-1, channel_multiplier=-1), reads=[blk2[:]], writes=[mask2[:, 1, :]])
    memset(half01[:], 0.0)
    memset(half01[0:64, 0, :], 1.0)
    memset(half01[64:128, 1, :], 1.0)
    amask = CA.a("amask", [128, 2, 128], BF)
    S.add("pool", lambda e: e.affine_select(out=amask[:, 0, :], in_=ones_b[:], pattern=[[-1, 128]], compare_op=ALU.is_ge,
                                            fill=0.0, base=0, channel_multiplier=1), reads=[ones_b[:]], writes=[amask[:, 0, :]])
    S.add("pool", lambda e: e.affine_select(out=amask[:, 1, :], in_=ones_b[:], pattern=[[1, 128]], compare_op=ALU.is_ge,
                                            fill=0.0, base=0, channel_multiplier=-1), reads=[ones_b[:]], writes=[amask[:, 1, :]])

    stg = WA.a("stg", [72, 128], F32)
    dma("sp", stg[0:8, :], c_d.ap().rearrange("(k p) -> k p", p=128), "st0")
    dma("sp", stg[8:16, :], g_mix_d.ap().rearrange("(k p) -> k p", p=128), "st1")
    dma("sp", stg[16:24, :], g_ffn_d.ap().rearrange("(k p) -> k p", p=128), "st2")
    dma("sp", stg[24:72, :], b_ada_d.ap().rearrange("(k p) -> k p", p=128), "st3")
    cols = CA.a("cols", [128, 72], F32)
    pb_ = bank()
    tr(pb_[:, 0:72], stg[:, :], ident[0:72, 0:72])
    cp(cols[:], pb_[:, 0:72])
    cs_b = CA.a("cs_b", [128, 8], BF)
    act(cs_b[:], cols[:, 0:8], AF.Silu)
    stg2 = WA.a("stg2", [96, 128], F32)
    dma("sp", stg2[:, :], conv_w_d.ap().rearrange("j (c p) -> (j c) p", p=128), "st4")
    convcol = CA.a("convcol", [128, 96], F32)
    pb_ = bank()
    tr(pb_[:, 0:96], stg2[:, :], ident[0:96, 0:96])
    cp(convcol[:], pb_[:, 0:96])
    rowc = CA.a("rowc", [128, 16 + 128 + 64], F32)
    dma("sp", rowc[:, 0:8], a_log_d.ap().partition_broadcast(128), "st5")
    dma("sp", rowc[:, 8:16], dt_bias_d.ap().partition_broadcast(128), "st6")
    dma("sp", rowc[:, 16:144], gdn_ng_d.ap().partition_broadcast(128), "st7")
    dma("sp", rowc[:, 144:208], r_bias_d.ap().partition_broadcast(128), "st8")
    dgcol = CA.a("dgcol", [128, 2], F32)
    dma("sp", dgcol[:, 0:1], dil_qg_d.ap().rearrange("(p o) -> p o", o=1), "st9")
    dma("sp", dgcol[:, 1:2], dil_kg_d.ap().rearrange("(p o) -> p o", o=1), "st10")
    ngcol = CA.a("ngcol", [128, 1], F32)
    dma("sp", ngcol[:, 0:1], gdn_ng_d.ap().rearrange("(p o) -> p o", o=1), "st11")

    hT = S.sb("hT", [128, 8, T], BF, off=B0 + 16 * KB)
    y_aT = S.sb("y_aT", [128, 8, T], BF, off=B0 + 48 * KB)
    y_bT = S.sb("y_bT", [128, 4, T], BF, off=B0 + 80 * KB)
    mod = CA.a("mod", [128, 48], F32)
    st1 = CA.a("st1", [128, 8], F32)
    wada = [WA.a("wada%d" % i, [128, 8, 512], BF) for i in range(2)]
    xt = [WA.a("xt%d" % i, [128, D], F32) for i in range(2)]
    xn = [WA.a("xn%d" % i, [128, D], F32) for i in range(2)]
    junk = WA.a("junk", [128, D], BF)
    xnT = WA.a("xnT", [128, 8, T], F32)
    pm = bacq()

    def x_tile(t):
        i = t % 2
        dma("sp", xt[i][:], x_d.ap()[t * 128:(t + 1) * 128, :], "xt%d" % i)
        ss = st1[:, 0:1]
        act(junk[:], xt[i][:], AF.Square, accum=ss)
        act(st1[:, 1:2], ss, AF.Sqrt, bias=EPS, scale=1.0 / D)
        recip(st1[:, 2:3], st1[:, 1:2])
        ts(xn[i][:], xt[i][:], st1[:, 2:3])
        for half in range(2):
            pt = bank()
            for q in range(4):
                kc = half * 4 + q
                tr(pt[:, q * 128:(q + 1) * 128], xn[i][:, kc * 128:(kc + 1) * 128], ident[:])
            cp(xnT[:, half * 4:(half + 1) * 4, t * 128:(t + 1) * 128],
               pt[:, :].rearrange("p (a b) -> p a b", b=128), eng=("act" if half == 0 else "dve"))

    tiles_at = [2, 1, 1, 2, 1, 1, 2, 1, 1, 2, 1, 1]
    tnext = 0
    for blk in range(12):
        wt = wada[blk % 2]
        dma("pool", wt[:], w_ada_d.ap()[:, blk * 512:(blk + 1) * 512].rearrange("(k p) n -> p k n", p=128), "wada%d" % (blk % 2))
        if blk >= 1:
            for _ in range(tiles_at[blk - 1]):
                x_tile(tnext)
                tnext += 1
        for j in range(4):
            col = blk * 4 + j
            for kc in range(8):
                mm(pm[:, col:col + 1], wt[:, kc, j * 128:(j + 1) * 128], cs_b[:, kc:kc + 1], start=(kc == 0), stop=(kc == 7))
    while tnext < NT:
        x_tile(tnext)
        tnext += 1
    tt(mod[:], pm[:, 0:48], cols[:, 24:72], ALU.add)
    brel(pm)
    AB = CA.a("AB", [128, 32], F32)
    stt(AB[:, 0:8], mod[:, 8:16], 1.0, cols[:, 8:16], ALU.add, ALU.mult)
    cp(AB[:, 8:16], mod[:, 0:8])
    stt(AB[:, 16:24], mod[:, 32:40], 1.0, cols[:, 16:24], ALU.add, ALU.mult)
    cp(AB[:, 24:32], mod[:, 24:32])
    for kc in range(8):
        if kc % 2 == 0:
            act(hT[:, kc, :], xnT[:, kc, :], AF.Identity, bias=AB[:, 8 + kc:9 + kc], scale=AB[:, kc:kc + 1])
        else:
            ts(hT[:, kc, :], xnT[:, kc, :], AB[:, kc:kc + 1], AB[:, 8 + kc:9 + kc], ALU.mult, ALU.add)
    gtB = CA.a("gtB", [128, 2, 1024], F32)
    dg = WA.a("dg", [128, 4, 128], F32)
    for gi, c0 in enumerate((16, 40)):
        for half in range(2):
            for q in range(4):
                kc = half * 4 + q
                ts(dg[:, q, :], ident[:], mod[:, c0 + kc:c0 + kc + 1])
            pg = bank()
            mm(pg[:, :], ones_f[:], dg[:].rearrange("p a b -> p (a b)"))
            cp(gtB[:, gi, half * 512:(half + 1) * 512], pg[:, :], eng="act")

    if "mod" in dbg:
        dump("mod", mod[:], [128, 48])
        dump("gtB", gtB[:], [128, 2, 1024])

    if "hT" in dbg:
        dump("hT", hT[:], [128, 8, T])
    if stage <= 1:
        return finish()

    S.barrier()
    WA.cur = WA.base
    wba = WA.a("wba", [128, 8, 16], BF)
    dma("pool", wba[:], w_in_d.ap()[:, OFF_GB:OFF_GB + 16].rearrange("(k p) n -> p k n", p=128), "wba")
    ba = WA.a("ba", [128, 16, 16], F32)
    pbk = bank()
    for t in range(NT):
        for kc in range(8):
            mm(pbk[:, t * 16:(t + 1) * 16], hT[:, kc, t * 128:(t + 1) * 128], wba[:, kc, :], start=(kc == 0), stop=(kc == 7))
    cp(ba[:], pbk[:, 0:256].rearrange("p (a b) -> p a b", b=16))
    NS = lambda nm: WA.a(nm, [128, 16, 8], F32)
    beta, lb, gg, gc, gcl, egc, edec, bg, egt0, egt1, tmp8 = [NS(n) for n in
        ("beta", "lb", "gg", "gc", "gcl", "egc", "edec", "bg", "egt0", "egt1", "tmp8")]
    nea = WA.a("nea", [128, 8], F32)
    act(beta[:], ba[:, :, 0:8], AF.Sigmoid)
    act(lb[:], beta[:], AF.Ln)
    tt(tmp8[:], ba[:, :, 8:16], rowc[:, None, 8:16].to_broadcast([128, 16, 8]), ALU.add)
    act(tmp8[:], tmp8[:], AF.Exp)
    act(tmp8[:], tmp8[:], AF.Ln, bias=1.0)
    act(nea[:], rowc[:, 0:8], AF.Exp)
    ts(nea[:], nea[:], -1.0)
    tt(gg[:], tmp8[:], nea[:, None, :].to_broadcast([128, 16, 8]), ALU.mult)
    pg = bank()
    g2 = gg[:].rearrange("p a b -> p (a b)")
    mm(pg[:, 0:128], tri2[:], g2)
    mm(pg[:, 128:256], blk2[:], g2)
    mm(pg[:, 256:384], half01[:, 0, :], g2)
    mm(pg[:, 384:512], half01[:, 1, :], g2)
    v3 = lambda ap: ap.rearrange("p (a b) -> p a b", b=8)
    cp(gc[:], v3(pg[:, 0:128]))
    tt(gcl[:], gc[:], lb[:], ALU.add)
    act(egc[:], gc[:], AF.Exp)
    tt(tmp8[:], v3(pg[:, 128:256]), gc[:], ALU.subtract)
    act(edec[:], tmp8[:], AF.Exp)
    tt(bg[:], beta[:], egc[:], ALU.mult)
    act(egt0[:], v3(pg[:, 256:384]), AF.Exp)
    act(egt1[:], v3(pg[:, 384:512]), AF.Exp)
    egt = (egt0, egt1)
    ngB = rowc[:, 16:144]

    if "gsc" in dbg:
        dump("beta", beta[:], [128, 16, 8])
        dump("gc", gc[:], [128, 16, 8])
        dump("egt0", egt0[:], [128, 16, 8])

    def so_set(i):
        d = {}
        d["qT"] = WA.a("so_qT%d" % i, [128, T], BF)
        d["wT"] = WA.a("so_wT%d" % i, [128, T], BF)
        d["u"] = WA.a("so_u%d" % i, [128, 16, 128], BF)
        d["qkT"] = WA.a("so_qkT%d" % i, [128, 16, 128], BF)
        d["kdec"] = WA.a("so_kdec%d" % i, [128, 16, 128], BF)
        d["zg"] = WA.a("so_zg%d" % i, [128, T], BF)
        d["nw"] = WA.a("so_nw%d" % i, [128, 16, 128], BF)
        return d

    SO = [so_set(0), so_set(1)]
    w4 = WA.a("w4", [128, 8, 4, 128], BF)
    xc = WA.a("xc", [128, 2048 + 16], BF)
    kT = WA.a("kT", [128, T], BF)
    vs = WA.a("vs", [128, T], BF)
    ktw = WA.a("ktw", [128, 16, 128], BF)
    vb = WA.a("vb", [128, 16, 128], BF)
    qs = WA.a("qs", [128, 512], F32)
    sq = WA.a("sq", [128, 512], BF)
    rt = WA.a("rt", [128, 512], F32)
    dgc = WA.a("dgc", [128, 4, 128], BF)
    NTT = 6
    for a_ in dbg:
        if a_.startswith("ntt"):
            NTT = int(a_[3:])
    TA = Arena(B0 + 80 * KB, B0 + 96 * KB)
    TMP = []
    for i in range(NTT):
        d = {}
        AR = TA if i >= 3 else WA
        d["X"] = AR.a("tX%d" % i, [128, 2, 128], F32)
        d["Dm"] = d["X"]
        d["E"] = d["X"]
        d["B"] = [AR.a("tB%d_%d" % (i, k), [128, 3, 128], F32) for k in range(2)]
        d["Gb"] = (WA if i == 5 else AR).a("tGb%d" % i, [128, 128], BF)
        TMP.append(d)
    Sf = TA.a("Sf", [128, 128], F32)
    negm = WA.a("negm", [128, 2, 128], F32)
    ts(negm[:], mask2[:], 1.0, 30000.0, ALU.subtract, ALU.mult)
    Sb2 = [TA.a("Sb2_%d" % i, [128, 128], BF) for i in range(2)]
    LA = 2
    RING = LA + 1
    MTr = [TA.a("MTr%d" % i, [128, 128], BF) for i in range(RING)]
    vn = TA.a("vn", [128, 128], BF)
    ot = [TA.a("ot%d" % i, [128, 128], F32) for i in range(2)]
    yt = TA.a("yt", [128, 128], BF)
    junkg = TA.a("junkg", [128, 128], BF)
    memset(xc[:, 0:3], 0.0)
    if "mem" in dbg:
        print("GDN WA used", (WA.cur - WA.base) / 1024, "of", (WA.limit - WA.base) / 1024, "TA used", (TA.cur - TA.base) / 1024)

    def gdn_tile(h, t, so, tm):
        X, Dm, E, B = tm["X"], tm["Dm"], tm["E"], tm["B"]
        tsl = slice(t * 128, (t + 1) * 128)
        ts(X[:, 0, :], ident[:], gc[:, t, h:h + 1])
        ts(X[:, 1, :], ident[:], gcl[:, t, h:h + 1])
        yield
        pR = bacq()
        mm(pR[:, 0:256], ones_f[:], X[:].rearrange("p a b -> p (a b)"))
        mm(pR[:, 256:384], kT[:, tsl], so["qT"][:, tsl])
        mm(pR[:, 384:512], kT[:, tsl], kT[:, tsl])
        yield
        stt(Dm[:].rearrange("p a b -> p (a b)"), pR[:, 0:256], gc[:, t, h:h + 1],
            negm[:].rearrange("p a b -> p (a b)"), ALU.subtract, ALU.add)
        yield
        act(E[:], Dm[:], AF.Exp)
        yield
        tt(so["qkT"][:, t, :], pR[:, 256:384], E[:, 0, :], ALU.mult)
        tt(B[0][:, 0, :], pR[:, 384:512], E[:, 1, :], ALU.mult)
        brel(pR)
        yield
        pT = bacq()
        tr(pT[:, 0:128], B[0][:, 0, :], ident[:])
        yield
        cp(B[0][:, 2, :], pT[:, 0:128], eng="act")
        tt(B[1][:, 1, :], ident[:], B[0][:, 0, :], ALU.subtract, eng="pool")
        brel(pT)
        yield
        pa = bacq()
        mm(pa[:, 0:128], B[0][:, 2, :], B[0][:, 0, :])
        mm(pa[:, 256:384], B[0][:, 0, :], B[0][:, 2, :])
        yield
        pav = pa[:, 0:384].rearrange("p (a b) -> p a b", b=128)
        cp(B[1][:, 0:3:2, :], pav[:, 0:3:2, :], eng="act")
        brel(pa)
        yield
        cur = 1
        for k in range(1, 5):
            nxt = 1 - cur
            pa = bacq()
            mm(pa[:, 0:256], B[cur][:, 2, :], B[cur][:, 0:2, :].rearrange("p a b -> p (a b)"))
            mm(pa[:, 256:384], B[cur][:, 0, :], B[cur][:, 2, :])
            yield
            pav = pa[:, 0:384].rearrange("p (a b) -> p a b", b=128)
            cp(B[nxt][:, 0:3:2, :], pav[:, 0:3:2, :], eng="act")
            tt(B[nxt][:, 1, :], B[cur][:, 1, :], pa[:, 128:256], ALU.add)
            brel(pa)
            cur = nxt
            yield
        pa = bacq()
        mm(pa[:, 0:128], B[cur][:, 2, :], B[cur][:, 1, :])
        yield
        G = tm["Gb"][:, :]
        tt(G, B[cur][:, 1, :], pa[:, 0:128], ALU.add)
        brel(pa)
        yield
        pu = bacq()
        mm(pu[:, 0:128], G, vb[:, t, :])
        mm(pu[:, 128:256], ktw[:, t, :], G)
        mm(pu[:, 256:384], G, ktw[:, t, :])
        yield
        cp(so["u"][:, t, :], pu[:, 0:128], eng="act")
        cp(so["wT"][:, tsl], pu[:, 128:256])
        ts(so["nw"][:, t, :], pu[:, 256:384], -1.0)
        brel(pu)
        yield

    def w4_load(h):
        for j, off in enumerate((OFF_GQ, OFF_GK, OFF_GV, OFF_GZ)):
            dma("pool", w4[:, :, j, :], w_in_d.ap()[:, off + h * 128: off + (h + 1) * 128].rearrange("(k p) n -> p k n", p=128), "w4_%d" % j)

    def gdn_pre(h, so):
        for jj in range(3):
            ct = jj * 8 + h
            for tap in range(4):
                ts(dgc[:, tap, :], ident[:], convcol[:, tap * 24 + ct: tap * 24 + ct + 1])
            for tb in range(4):
                pb = bank()
                for kc in range(8):
                    mm(pb[:, :], w4[:, kc, jj, :], hT[:, kc, tb * 512:(tb + 1) * 512], start=(kc == 0), stop=(kc == 7))
                cp(xc[:, 3 + tb * 512: 3 + (tb + 1) * 512], pb[:, :], eng="act")
                if tb % 2 == 1:
                    yield
            for tb in range(4):
                pb = bank()
                for tap in range(4):
                    mm(pb[:, :], dgc[:, tap, :], xc[:, tb * 512 + tap: tb * 512 + tap + 512], start=(tap == 0), stop=(tap == 3))
                bsl = slice(tb * 512, (tb + 1) * 512)
                if jj == 2:
                    act(vs[:, bsl], pb[:, :], AF.Silu)
                else:
                    act(qs[:], pb[:, :], AF.Silu)
                    act(sq[:], qs[:], AF.Square)
                    p2 = bank()
                    mm(p2[:, :], ones_b[:], sq[:])
                    act(rt[:], p2[:, :], AF.Sqrt, bias=EPS)
                    recip(rt[:], rt[:])
                    if jj == 0:
                        stt(so["qT"][:, bsl], qs[:], float(128 ** -0.5), rt[:], ALU.mult, ALU.mult)
                    else:
                        tt(kT[:, bsl], qs[:], rt[:], ALU.mult)
                if tb % 2 == 1:
                    yield
        for tb in range(4):
            pb = bank()
            for kc in range(8):
                mm(pb[:, :], w4[:, kc, 3, :], hT[:, kc, tb * 512:(tb + 1) * 512], start=(kc == 0), stop=(kc == 7))
            act(qs[:], pb[:, :], AF.Silu)
            ts(so["zg"][:, tb * 512:(tb + 1) * 512], qs[:], ngcol[:, 0:1])
            if tb % 2 == 1:
                yield
        if h + 1 < NH:
            w4_load(h + 1)
        yield
        for g4 in range(4):
            pb = bank()
            pv = bf(pb[:, :])
            for q in range(4):
                t = g4 * 4 + q
                tr(pv[:, q * 128:(q + 1) * 128], kT[:, t * 128:(t + 1) * 128], identb[:])
                tr(pv[:, 512 + q * 128: 512 + (q + 1) * 128], vs[:, t * 128:(t + 1) * 128], identb[:])
            kv_ = pv[:, 0:512].rearrange("p (a b) -> p a b", b=128)
            vv_ = pv[:, 512:1024].rearrange("p (a b) -> p a b", b=128)
            g4s = slice(g4 * 4, (g4 + 1) * 4)
            tt(ktw[:, g4s, :], kv_, bg[:, g4s, h:h + 1].to_broadcast([128, 4, 128]), ALU.mult)
            tt(so["kdec"][:, g4s, :], kv_, edec[:, g4s, h:h + 1].to_broadcast([128, 4, 128]), ALU.mult)
            tt(vb[:, g4s, :], vv_, beta[:, g4s, h:h + 1].to_broadcast([128, 4, 128]), ALU.mult)
            yield
        pend = list(range(NT if "notile" not in dbg else 0))
        active = []
        free_tmp = list(range(NTT))
        while pend or active:
            if pend and free_tmp:
                ti = free_tmp.pop(0)
                active.append((gdn_tile(h, pend.pop(0), so, TMP[ti]), ti))
            nxt = []
            for g, ti in active:
                try:
                    next(g)
                    nxt.append((g, ti))
                except StopIteration:
                    free_tmp.append(ti)
            active = nxt
            yield

    def gdn_scan(h, so):
        memset(Sf[:], 0.0)
        memset(Sb2[0][:], 0.0)
        NS_ = 32

        def rng(n):
            t, half = divmod(n, 2)
            lo, hi = half * 64, half * 64 + 64
            return t, half, lo, hi

        for n0 in range(LA):
            t, half, lo, hi = rng(n0)
            pM = bacq()
            mm(pM[:, 0:128], so["nw"][lo:hi, t, :], so["kdec"][lo:hi, t, :])
            yield
            cp(MTr[n0 % RING][:], pM[:, 0:128], eng="act")
            brel(pM)
            yield
        later = {}

        def at(r, fn):
            later.setdefault(r, []).append(fn)

        pY = [None]

        def getY():
            if pY[0] is None:
                pY[0] = bacq()
            return pY[0]

        def fin_a(t):
            o_ = ot[t % 2]
            act(junkg[:], o_[:], AF.Square, accum=st1[:, 3:4])
            act(st1[:, 4:5], st1[:, 3:4], AF.Ln, bias=EPS, scale=1.0 / 128)
            act(st1[:, 5:6], st1[:, 4:5], AF.Exp, scale=-0.5)

        def fin_b(t):
            o_ = ot[t % 2]
            ts(yt[:], o_[:], st1[:, 5:6])

        def fin_c(t):
            py = getY()
            tr(bf(py[:, :])[:, 512:640], yt[:], identb[:])

        def fin_d(t):
            py = getY()
            tt(y_aT[:, h, t * 128:(t + 1) * 128], bf(py[:, :])[:, 512:640], so["zg"][:, t * 128:(t + 1) * 128], ALU.mult)

        prev = None
        r = 0
        n = 0
        while n < NS_ or prev is not None or any(k >= r for k in later):
            pX = None
            cur = None
            if n < NS_:
                t, half, lo, hi = rng(n)
                tok = slice(t * 128 + lo, t * 128 + hi)
                Sc = Sb2[n % 2]
                pX = bacq()
                mm(pX[:, 256:384], so["kdec"][lo:hi, t, :], so["u"][lo:hi, t, :], start=True, stop=False)
                mm(pX[:, 256:384], MTr[n % RING][:], Sc[:, :], start=False, stop=True)
                mm(pX[lo:hi, 0:128], so["wT"][:, tok], Sc[:, :])
                mm(pX[lo:hi, 128:256], so["qT"][:, tok], Sc[:, :])
                cur = (n, t, half, lo, hi)
            if prev is not None:
                pn, pt_, phalf, plo, phi = prev
                if pX is None:
                    pX = bacq()
                mm(pX[plo:phi, 384:512], so["qkT"][plo:phi, pt_, plo:phi], vn[plo:phi, :])
            nla = n + LA
            if nla < NS_:
                t2, half2, lo2, hi2 = rng(nla)
                py = getY()
                mm(py[:, 0:128], so["nw"][lo2:hi2, t2, :], so["kdec"][lo2:hi2, t2, :])
            for fn in later.pop(r, []):
                fn()
            r += 1
            yield
            if cur is not None:
                Sn = Sb2[(n + 1) % 2]
                stt(Sn[:], Sf[:], egt[half][:, t, h:h + 1], pX[:, 256:384], ALU.mult, ALU.add)
                stt(Sf[:], Sf[:], egt[half][:, t, h:h + 1], pX[:, 256:384], ALU.mult, ALU.add)
            if prev is not None:
                pn, pt_, phalf, plo, phi = prev
                po_ = ot[pt_ % 2]
                tt(po_[plo:phi, :], po_[plo:phi, :], pX[plo:phi, 384:512], ALU.add)
                if phalf == 1:
                    fin_a(pt_)
                    at(r + 2, lambda pt_=pt_: fin_b(pt_))
                    at(r + 3, lambda pt_=pt_: fin_c(pt_))
                    at(r + 4, lambda pt_=pt_: fin_d(pt_))
            if cur is not None:
                o_ = ot[t % 2]
                tt(vn[lo:hi, :], so["u"][lo:hi, t, :], pX[lo:hi, 0:128], ALU.subtract)
                act(o_[lo:hi, :], pX[lo:hi, 128:256], AF.Copy, scale=egc[lo:hi, t, h:h + 1])
            if pX is not None:
                brel(pX)
            if nla < NS_:
                cp(MTr[nla % RING][:], pY[0][:, 0:128], eng="act")
            for fn in later.pop(r, []):
                fn()
            if pY[0] is not None:
                brel(pY[0])
                pY[0] = None
            r += 1
            prev = cur
            n += 1
            yield

    def multi(g, k):
        while True:
            for _ in range(k):
                try:
                    next(g)
                except StopIteration:
                    return
            yield

    NH = 8 if "gdn1" not in dbg else 1
    if "gdn2" in dbg:
        NH = 2
    w4_load(0)
    run_threads([gdn_pre(0, SO[0])])
    for h in range(NH):
        SM = 1
        for a_ in dbg:
            if a_.startswith("sm"):
                SM = int(a_[2:])
        th = [multi(gdn_scan(h, SO[h % 2]), SM)]
        if "noil" in dbg:
            run_threads(th)
            th = []
        if h + 1 < NH:
            th.append(gdn_pre(h + 1, SO[(h + 1) % 2]))
        run_threads(th)

    if "gdn" in dbg:
        dump("y_aT", y_aT[:, 0:NH, :], [128, NH, T])
        sd = SO[(NH - 1) % 2]
        dump("so_qT", sd["qT"][:], [128, T])
        dump("kT", kT[:], [128, T])
        dump("so_u", sd["u"][:], [128, 16, 128])
        dump("so_wT", sd["wT"][:], [128, T])
        dump("so_qkT", sd["qkT"][:], [128, 16, 128])
        dump("so_kdec", sd["kdec"][:], [128, 16, 128])
        dump("ktw", ktw[:], [128, 16, 128])
        dump("vb", vb[:], [128, 16, 128])
        dump("bg", bg[:], [128, 16, 8])
    if stage <= 2:
        return finish()

    S.barrier()
    WA.cur = WA.base
    ts(dgcol[:, 0:1], dgcol[:, 0:1], float(128 ** -0.5))
    DB = []
    for i in range(2):
        d = {}
        d["w"] = WA.a("dw%d" % i, [128, 8, 3, 128], BF)
        d["q"] = WA.a("dq%d" % i, [128, T], BF)
        d["k"] = WA.a("dk%d" % i, [128, T], BF)
        d["vt"] = WA.a("dvt%d" % i, [128, 16, 128], BF)
        DB.append(d)
    dvT = WA.a("dvT", [128, T], BF)
    sqd = WA.a("sqd", [128, 512], BF)
    rtd = WA.a("rtd", [128, 512], F32)
    acc = WA.a("acc", [128, 2, T], F32)
    Pf = [WA.a("Pf%d" % i, [128, 2, 128], BF) for i in range(6)]
    PTb = [WA.a("PTb%d" % i, [128, 2, 128], BF) for i in range(6)]
    DILS = (1, 4, 16)

    def dil_w_load(n):
        j, g = divmod(n, 3)
        hd = g * 4 + j
        for jj, off in enumerate((OFF_DQ, OFF_DK, OFF_DV)):
            dma("pool", DB[n % 2]["w"][:, :, jj, :], w_in_d.ap()[:, off + hd * 128: off + (hd + 1) * 128].rearrange("(k p) n -> p k n", p=128), "dw%d_%d" % (n % 2, jj))

    def dil_pre(n, db):
        j, g = divmod(n, 3)
        hd = g * 4 + j
        dil = DILS[g]
        if n + 1 < NDH:
            dil_w_load(n + 1)
        mb = 512 // dil
        for jj in range(3):
            dst = (db["q"], db["k"], dvT)[jj]
            for tb in range(4):
                pb = bank()
                for kc in range(8):
                    mm(pb[:, :], db["w"][:, kc, jj, :], hT[:, kc, tb * 512:(tb + 1) * 512], start=(kc == 0), stop=(kc == 7))
                if dil == 1:
                    dview = dst[:, tb * 512:(tb + 1) * 512]
                    sview = pb[:, :]
                    rview = rtd[:, :]
                else:
                    dview = dst[:, :].rearrange("p (r m) -> p r m", r=dil)[:, :, tb * mb:(tb + 1) * mb]
                    sview = pb[:, :].rearrange("p (m r) -> p r m", r=dil)
                    rview = rtd[:, :].rearrange("p (m r) -> p r m", r=dil)
                if jj == 2:
                    cp(dview, sview, eng="act")
                else:
                    act(sqd[:], pb[:, :], AF.Square)
                    p2 = bank()
                    mm(p2[:, :], ones_b[:], sqd[:])
                    act(rtd[:], p2[:, :], AF.Ln, bias=EPS, scale=1.0 / 128)
                    act(rtd[:], rtd[:], AF.Exp, scale=-0.5)
                    stt(dview, sview, dgcol[:, jj:jj + 1], rview, ALU.mult, ALU.mult)
                if True:
                    yield
        for g4 in range(4):
            pb = bank()
            pv = bf(pb[:, :])
            for q in range(4):
                t = g4 * 4 + q
                tr(pv[:, q * 128:(q + 1) * 128], dvT[:, t * 128:(t + 1) * 128], identb[:])
            cp(db["vt"][:, g4 * 4:(g4 + 1) * 4, :], pv[:, 0:512].rearrange("p (a b) -> p a b", b=128), eng="act")
            yield

    NDT = 6

    def dil_qtile(n, db, tq, ti):
        j, g = divmod(n, 3)
        dil = DILS[g]
        ntr = 16 // dil
        r, mt = divmod(tq, ntr)
        qs_ = slice(tq * 128, (tq + 1) * 128)
        P_, PT_ = Pf[ti], PTb[ti]
        pS = bacq()
        if mt > 0:
            mm(pS[:, 0:128], db["k"][:, (tq - 1) * 128: tq * 128], db["q"][:, qs_])
        mm(pS[:, 128:256], db["k"][:, qs_], db["q"][:, qs_])
        yield
        if mt > 0:
            act(P_[:].rearrange("p a b -> p (a b)"), pS[:, 0:256], AF.Exp)
        else:
            act(P_[:, 1, :], pS[:, 128:256], AF.Exp)
        brel(pS)
        yield
        if mt > 0:
            tt(PT_[:], P_[:], amask[:], ALU.mult, eng="pool")
        else:
            tt(PT_[:, 1, :], P_[:, 1, :], amask[:, 1, :], ALU.mult, eng="pool")
        yield
        pN = bacq()
        if mt > 0:
            mm(pN[:, 0:128], db["vt"][:, tq - 1, :], PT_[:, 0, :], start=True, stop=False)
        mm(pN[:, 0:128], db["vt"][:, tq, :], PT_[:, 1, :], start=(mt == 0), stop=True)
        if mt > 0:
            mm(pN[:, 128:256], ones_b[:], PT_[:, 0, :], start=True, stop=False)
        mm(pN[:, 128:256], ones_b[:], PT_[:, 1, :], start=(mt == 0), stop=True)
        yield
        st = r + mt * 128 * dil
        aview = acc[:, :, st: st + 127 * dil + 1: dil]
        nview = pN[:, 0:256].rearrange("p (a b) -> p a b", b=128)
        if g == 0:
            cp(aview, nview, eng="act")
        else:
            tt(aview, aview, nview, ALU.add)
        brel(pN)
        yield

    def dil_att(n, db):
        j, g = divmod(n, 3)
        pend = list(range(16))
        active = []
        free_t = list(range(NDT))
        while pend or active:
            if pend and free_t:
                ti = free_t.pop(0)
                active.append((dil_qtile(n, db, pend.pop(0), ti), ti))
            nxt = []
            for gq, ti in active:
                try:
                    next(gq)
                    nxt.append((gq, ti))
                except StopIteration:
                    free_t.append(ti)
            active = nxt
            yield
        if g == 2:
            recip(acc[:, 1, :], acc[:, 1, :])
            tt(y_bT[:, j, :], acc[:, 0, :], acc[:, 1, :], ALU.mult)
            yield

    NDH = 12 if "dil1" not in dbg else 3
    dil_w_load(0)
    run_threads([dil_pre(0, DB[0])])
    for n in range(NDH):
        th = [dil_att(n, DB[n % 2])]
        if n + 1 < NDH:
            th.append(dil_pre(n + 1, DB[(n + 1) % 2]))
        run_threads(th)
    if "dil" in dbg:
        dump("y_bT", y_bT[:, 0:NDH // 3, :], [128, NDH // 3, T])
    if stage <= 3:
        return finish()

    S.barrier()
    WA.cur = WA.base
    mergedT = WA.a("mergedT", [128, 8, T], BF)
    wg2 = [WA.a("wg2_%d" % i, [128, 8, 2, 128], BF) for i in range(2)]
    wupa = [WA.a("wupa%d" % i, [128, 8, 128], BF) for i in range(2)]
    wupd = [WA.a("wupd%d" % i, [128, 4, 128], BF) for i in range(2)]
    sga = [WA.a("sga%d" % i, [128, 512], F32) for i in range(2)]
    sgb = [WA.a("sgb%d" % i, [128, 512], F32) for i in range(2)]
    m1 = [WA.a("m1_%d" % i, [128, 512], BF) for i in range(2)]
    m2 = [WA.a("m2_%d" % i, [128, 512], BF) for i in range(2)]
    wo = WA.a("wo", [128, 8, D], BF)
    xt2 = [WA.a("xt2_%d" % i, [128, D], F32) for i in range(2)]
    for ft in range(8):
        i = ft % 2
        fsl = slice(ft * 128, (ft + 1) * 128)
        dma("pool", wg2[i][:, :, 0, :], w_in_d.ap()[:, OFF_GA_GATE + ft * 128: OFF_GA_GATE + (ft + 1) * 128].rearrange("(k p) n -> p k n", p=128), "wg2a%d" % i)
        dma("pool", wg2[i][:, :, 1, :], w_in_d.ap()[:, OFF_GB_GATE + ft * 128: OFF_GB_GATE + (ft + 1) * 128].rearrange("(k p) n -> p k n", p=128), "wg2b%d" % i)
        dma("pool", wupa[i][:], w_upg_d.ap()[:, fsl].rearrange("(k p) n -> p k n", p=128), "wupa%d" % i)
        dma("pool", wupd[i][:], w_upd_d.ap()[:, fsl].rearrange("(k p) n -> p k n", p=128), "wupd%d" % i)
        for tb in range(4):
            k2 = tb % 2
            bsl = slice(tb * 512, (tb + 1) * 512)
            pa, pb, pc, pd = bank(), bank(), bank(), bank()
            for kc in range(8):
                mm(pa[:, :], wg2[i][:, kc, 0, :], hT[:, kc, bsl], start=(kc == 0), stop=(kc == 7))
            for kc in range(8):
                mm(pb[:, :], wg2[i][:, kc, 1, :], hT[:, kc, bsl], start=(kc == 0), stop=(kc == 7))
            for hc in range(8):
                mm(pc[:, :], wupa[i][:, hc, :], y_aT[:, hc, bsl], start=(hc == 0), stop=(hc == 7))
            for sc_ in range(4):
                mm(pd[:, :], wupd[i][:, sc_, :], y_bT[:, sc_, bsl], start=(sc_ == 0), stop=(sc_ == 3))
            act(sga[k2][:], pa[:, :], AF.Sigmoid)
            act(sgb[k2][:], pb[:, :], AF.Sigmoid)
            tt(m1[k2][:], sga[k2][:], pc[:, :], ALU.mult)
            tt(m2[k2][:], sgb[k2][:], pd[:, :], ALU.mult)
            tt(mergedT[:, ft, bsl], m1[k2][:], m2[k2][:], ALU.add)
    dma("pool", wo[:], w_out_d.ap().rearrange("(k p) n -> p k n", p=128), "wo")
    tt(wo[:], wo[:], gtB[:, 0:1, :].to_broadcast([128, 8, D]), ALU.mult, eng="pool")
    if "mrg" in dbg:
        dump("mergedT", mergedT[:], [128, 8, T])
    S.barrier()
    x1 = S.sb("x1", [128, 16, D], F32, off=B0 + 16 * KB)
    for t in range(NT):
        i = t % 2
        dma("sp", xt2[i][:], x_d.ap()[t * 128:(t + 1) * 128, :], "xt2_%d" % i)
        for half in range(2):
            hs = slice(half * 512, (half + 1) * 512)
            pb = bank()
            for ft in range(8):
                mm(pb[:, :], mergedT[:, ft, t * 128:(t + 1) * 128], wo[:, ft, hs], start=(ft == 0), stop=(ft == 7))
            tt(x1[:, t, hs], xt2[i][:, hs], pb[:, :], ALU.add)
    if "x1" in dbg:
        dump("x1", x1[:], [128, 16, D])
    if stage <= 4:
        return finish()

    S.barrier()
    MA = Arena(B0 + 80 * KB, B0 + 207 * KB)
    h2T = MA.a("h2T", [128, 8, T], BF)
    cT = MA.a("cT", [64, T], BF)
    wgu = [MA.a("wgu%d" % i, [128, 8, 512], BF) for i in range(3)]
    wdn = [MA.a("wdn%d" % i, [128, 2, D], BF) for i in range(6)]
    sact = [MA.a("sact%d" % i, [128, 512], BF) for i in range(2)]
    tmid = [MA.a("tmid%d" % i, [128, 512], BF) for i in range(2)]
    sel_e = [MA.a("sel_e%d" % i, [64, 128], BF) for i in range(2)]
    mark = MA.cur
    hTe = [[MA.a("hTe%d_%d" % (a, b), [128, 2, T], BF) for b in range(2)] for a in range(2)]
    MA.cur = mark
    wr = MA.a("wr", [128, 8, 64], F32)
    dma("sp", wr[:], w_r_d.ap().rearrange("(k p) n -> p k n", p=128), "wr")
    rbias = rowc[:, 144:208]
    NRT = 3
    RT = []
    for i in range(NRT):
        d = {}
        d["xn"] = MA.a("xn2_%d" % i, [128, D], F32)
        d["junk"] = MA.a("junk2_%d" % i, [128, D], BF)
        d["h2f"] = MA.a("h2f_%d" % i, [128, 8, 128], F32)
        for nm in ("scr", "sel", "selm", "wkk", "comb", "emk"):
            d[nm] = MA.a("%s_%d" % (nm, i), [128, 64], F32)
        d["top"] = MA.a("top_%d" % i, [128, 8, 8], F32)
        for nm in ("gs", "gtop", "gm", "t30", "top8", "st"):
            d[nm] = MA.a("%s_%d" % (nm, i), [128, 8], F32)
        RT.append(d)

    def router_tile(t, d):
        src_ap = x1[:, t, :]
        st_ = d["st"]
        xn_, h2f = d["xn"], d["h2f"]
        act(d["junk"][:], src_ap, AF.Square, accum=st_[:, 0:1])
        yield
        act(st_[:, 1:2], st_[:, 0:1], AF.Sqrt, bias=EPS, scale=1.0 / D)
        yield
        recip(st_[:, 2:3], st_[:, 1:2])
        yield
        ts(xn_[:], src_ap, st_[:, 2:3])
        yield
        for half in range(2):
            pt = bacq()
            for q in range(4):
                kc = half * 4 + q
                tr(pt[:, q * 128:(q + 1) * 128], xn_[:, kc * 128:(kc + 1) * 128], ident[:])
            yield
            for q in range(4):
                kc = half * 4 + q
                if q % 2 == 0:
                    act(h2f[:, kc, :], pt[:, q * 128:(q + 1) * 128], AF.Identity,
                        bias=AB[:, 24 + kc:25 + kc], scale=AB[:, 16 + kc:17 + kc])
                else:
                    ts(h2f[:, kc, :], pt[:, q * 128:(q + 1) * 128], AB[:, 16 + kc:17 + kc],
                       AB[:, 24 + kc:25 + kc], ALU.mult, ALU.add)
            brel(pt)
            yield
            cp(h2T[:, half * 4:(half + 1) * 4, t * 128:(t + 1) * 128], h2f[:, half * 4:(half + 1) * 4, :], eng="pool")
        pl = bacq()
        for kc in range(8):
            mm(pl[:, 0:64], h2f[:, kc, :], wr[:, kc, :], start=(kc == 0), stop=(kc == 7))
        yield
        scr, sel, selm, wkk, comb, emk = [d[n] for n in ("scr", "sel", "selm", "wkk", "comb", "emk")]
        top, gs, gtop, gm, t30, top8 = [d[n] for n in ("top", "gs", "gtop", "gm", "t30", "top8")]
        act(scr[:], pl[:, 0:64], AF.Sigmoid)
        brel(pl)
        yield
        tt(sel[:], scr[:], rbias, ALU.add)
        yield
        for g in range(8):
            S.add("dve", lambda e, g=g: e.max(out=top[:, g, :], in_=sel[:, g * 8:(g + 1) * 8]),
                  reads=[sel[:, g * 8:(g + 1) * 8]], writes=[top[:, g, :]])
        yield
        tt(gs[:], top[:, :, 0], top[:, :, 1], ALU.add)
        yield
        S.add("dve", lambda e: e.max(out=gtop[:], in_=gs[:]), reads=[gs[:]], writes=[gtop[:]])
        yield
        ts(gm[:], gs[:], gtop[:, 3:4], None, ALU.is_ge)
        yield
        ts(t30[:], gm[:], 30.0, -30.0, ALU.mult, ALU.add)
        sel3 = sel[:].rearrange("p (a b) -> p a b", b=8)
        selm3 = selm[:].rearrange("p (a b) -> p a b", b=8)
        tt(selm3, sel3, gm[:, :, None].to_broadcast([128, 8, 8]), ALU.mult)
        yield
        tt(selm3, selm3, t30[:, :, None].to_broadcast([128, 8, 8]), ALU.add)
        yield
        S.add("dve", lambda e: e.max(out=top8[:], in_=selm[:]), reads=[selm[:]], writes=[top8[:]])
        yield
        ts(emk[:], selm[:], top8[:, 7:8], None, ALU.is_ge)
        yield
        tt(wkk[:], scr[:], emk[:], ALU.mult)
        yield
        S.add("dve", lambda e: e.reduce_sum(out=st_[:, 3:4], in_=wkk[:], axis=AX.X), reads=[wkk[:]], writes=[st_[:, 3:4]])
        yield
        recip(st_[:, 4:5], st_[:, 3:4])
        yield
        ts(comb[:], wkk[:], st_[:, 4:5], 2.5, ALU.mult, ALU.mult)
        yield
        pc = bacq()
        tr(pc[0:64, 0:128], comb[:], ident[:])
        yield
        cp(cT[:, t * 128:(t + 1) * 128], pc[0:64, 0:128], eng="act")
        brel(pc)
        yield

    groups = [(2 * g, 2 * g + 1) for g in range(32)] + [(64,)]
    if "moe1" in dbg:
        groups = groups[:2] + [(64,)]
    elist = [e for grp in groups for e in grp]

    def load_w(e, slot):
        wb = wgu[slot % 3]
        wd_ = wdn[slot % 6]
        if e < 64:
            dma("pool", wb[:, :, 0:256], w_eg_d.ap()[e].rearrange("(k p) n -> p k n", p=128), "wgu_g%d" % (slot % 3))
            dma("pool", wb[:, :, 256:512], w_eu_d.ap()[e].rearrange("(k p) n -> p k n", p=128), "wgu_u%d" % (slot % 3))
            dma("pool", wd_[:], w_ed_d.ap()[e].rearrange("(k p) n -> p k n", p=128), "wdn%d" % (slot % 6))
        else:
            dma("pool", wb[:, :, 0:256], w_sg_d.ap().rearrange("(k p) n -> p k n", p=128), "wgu_g%d" % (slot % 3))
            dma("pool", wb[:, :, 256:512], w_su_d.ap().rearrange("(k p) n -> p k n", p=128), "wgu_u%d" % (slot % 3))
            dma("pool", wd_[:], w_sd_d.ap().rearrange("(k p) n -> p k n", p=128), "wdn%d" % (slot % 6))

    load_w(elist[0], 0)
    load_w(elist[1], 1)

    pend_t = list(range(NT))
    active = []
    free_r = list(range(NRT))
    while pend_t or active:
        if pend_t and free_r:
            ri = free_r.pop(0)
            active.append((router_tile(pend_t.pop(0), RT[ri]), ri))
        nxt_ = []
        for g_, ri in active:
            try:
                next(g_)
                nxt_.append((g_, ri))
            except StopIteration:
                free_r.append(ri)
        active = nxt_
    if "rt" in dbg:
        dump("h2T", h2T[:], [128, 8, T])
        dump("cT", cT[:], [64, T])
    if stage <= 5:
        return finish()

    S.barrier()


    def prep_w(e, slot):
        wd_ = wdn[slot % 6]
        tt(wd_[:], wd_[:], gtB[:, 1:2, :].to_broadcast([128, 2, D]), ALU.mult)
        if e < 64:
            ts(sel_e[slot % 2][:], ones_b[0:64, :], ident[0:64, e:e + 1])

    def gateup(e, slot, gi):
        wb = wgu[slot % 3]
        wd_ = wdn[slot % 6]
        if slot + 2 < len(elist):
            load_w(elist[slot + 2], slot + 2)
        he = hTe[gi % 2][slot % 2]
        se = sel_e[slot % 2]
        for tb in range(4):
            if tb == 1 and slot + 1 < len(elist):
                prep_w(elist[slot + 1], slot + 1)
            bsl = slice(tb * 512, (tb + 1) * 512)
            if e < 64:
                pcb = bank()
                mm(pcb[:, :], se[:], cT[:, bsl])
            for ft in range(2):
                k2 = (tb * 2 + ft) % 2
                pa, pu = bank(), bank()
                for kc in range(8):
                    mm(pa[:, :], wb[:, kc, ft * 128:(ft + 1) * 128], h2T[:, kc, bsl], start=(kc == 0), stop=(kc == 7))
                for kc in range(8):
                    mm(pu[:, :], wb[:, kc, 256 + ft * 128: 256 + (ft + 1) * 128], h2T[:, kc, bsl], start=(kc == 0), stop=(kc == 7))
                act(sact[k2][:], pa[:, :], AF.Silu)
                if e < 64:
                    tt(tmid[k2][:], sact[k2][:], pu[:, :], ALU.mult)
                    tt(he[:, ft, bsl], tmid[k2][:], pcb[:, :], ALU.mult)
                else:
                    tt(he[:, ft, bsl], sact[k2][:], pu[:, :], ALU.mult)
        return he, wd_

    def down(items, last):
        for t in range(NT):
            for half in range(2):
                hs = slice(half * 512, (half + 1) * 512)
                pb = bank()
                n = len(items) * 2
                k = 0
                for (he, wd_) in items:
                    for ft in range(2):
                        mm(pb[:, :], he[:, ft, t * 128:(t + 1) * 128], wd_[:, ft, hs], start=(k == 0), stop=(k == n - 1))
                        k += 1
                tt(x1[:, t, hs], x1[:, t, hs], pb[:, :], ALU.add)
            if last:
                finals.append(dma("sp", out_d.ap()[t * 128:(t + 1) * 128, :], x1[:, t, :], "out%d" % (t % 4)))

    prep_w(elist[0], 0)
    slot = 0
    prev = None
    for gi, grp in enumerate(groups):
        items = []
        for e in grp:
            items.append(gateup(e, slot, gi))
            slot += 1
        if prev is not None:
            down(prev, False)
        prev = items
    down(prev, True)
    return finish()


_IN_NAMES = ["x", "c", "w_ada", "b_ada", "g_mix", "w_in", "gdn_conv_w", "gdn_a_log", "gdn_dt_bias", "gdn_norm_g",
             "dil_q_norm_g", "dil_k_norm_g", "w_up_gdn", "w_up_dil", "w_out", "g_ffn", "w_router", "router_bias",
             "w_exp_gate", "w_exp_up", "w_exp_down", "w_sh_gate", "w_sh_up", "w_sh_down"]


def make_in_maps(inputs, n_cores=8):
    shared = {}
    for k in _IN_NAMES:
        if k in ("x", "c"):
            continue
        a = np.asarray(inputs[k], dtype=np.float32)
        shared[k] = np.ascontiguousarray(a[0])
    maps = []
    for b in range(n_cores):
        m = dict(shared)
        m["x"] = np.ascontiguousarray(np.asarray(inputs["x"], dtype=np.float32)[b])
        m["c"] = np.ascontiguousarray(np.asarray(inputs["c"], dtype=np.float32)[b])
        maps.append(m)
    return maps


def kernel(**inputs):
    nc, _ = build()
    maps = make_in_maps(inputs)
    res = run_bass_kernel_spmd(nc, maps, core_ids=list(range(8)))
    out = np.stack([np.asarray(r["out"], dtype=np.float32) for r in res.results], axis=0)
    return out
```
